# Optimizing a Trainium2 kernel written in Bass

```python
import jax, jax.numpy as jnp
from jax import lax
import numpy as np

D_MODEL = 2048
BATCH = 2
SEQ = 8192
DEPTH = 1

CHUNK = 64
M_WIDTH = D_MODEL // 2
R_WIDTH = D_MODEL - M_WIDTH
M_HEADS = 4
M_HEAD_DIM = M_WIDTH // M_HEADS
R_BLOCKS = 8
R_BLOCK_DIM = R_WIDTH // R_BLOCKS
CONV_WIDTH = 4
LRU_C = 8.0
N_GROUPS = 4
EXPERTS_PER_GROUP = 8
N_EXPERTS = N_GROUPS * EXPERTS_PER_GROUP
TOP_K = 2
D_EXPERT = D_MODEL // 4
MOE_BLOCK = 128
EPS = 1e-6
IN_SPLITS = [M_WIDTH, M_WIDTH, M_WIDTH, M_WIDTH, M_HEADS, M_HEADS, R_WIDTH, R_WIDTH]
IN_WIDTH = sum(IN_SPLITS)

kernel_name = 'hybrid_mlstm_rglru_hmoe_block'


def rmsnorm(x):
    x32 = x.astype(jnp.float32)
    return (x32 * lax.rsqrt(jnp.mean(x32 * x32, axis=-1, keepdims=True) + EPS)).astype(x.dtype)


def causal_dwconv(x, w):
    return lax.conv_general_dilated(
        x, w[:, None, :].astype(x.dtype), window_strides=(1,),
        padding=[(CONV_WIDTH - 1, 0)], dimension_numbers=('NWC', 'WIO', 'NWC'),
        feature_group_count=x.shape[-1])


def mlstm_chunkwise(q, k, v, ig, lf):
    B, S, H, Dh = q.shape
    nc = S // CHUNK

    def to_chunks(t):
        t = t.reshape((B, nc, CHUNK, H) + t.shape[3:])
        return jnp.moveaxis(t, (1, 3), (0, 2))

    causal = jnp.tril(jnp.ones((CHUNK, CHUNK), dtype=bool))

    def step(carry, xs):
        C, n, m = carry
        qc, kc, vc, ic, fc = xs
        b = jnp.cumsum(fc, axis=-1)
        b_last = b[..., -1]
        dmat = b[..., :, None] - b[..., None, :] + ic[..., None, :]
        dmat = jnp.where(causal, dmat, -jnp.inf)
        inter = b + m[..., None]
        m_t = jnp.maximum(inter, jnp.max(dmat, axis=-1))
        s = jnp.einsum('bhtd,bhsd->bhts', qc, kc) * jnp.exp(dmat - m_t[..., None])
        e_inter = jnp.exp(inter - m_t)
        num = jnp.einsum('bhts,bhse->bhte', s, vc) + e_inter[..., None] * jnp.einsum('bhtd,bhde->bhte', qc, C)
        den = jnp.sum(s, axis=-1) + e_inter * jnp.einsum('bhtd,bhd->bht', qc, n)
        h = num / jnp.maximum(jnp.abs(den), jnp.exp(-m_t))[..., None]
        g = b_last[..., None] - b + ic
        m_new = jnp.maximum(b_last + m, jnp.max(g, axis=-1))
        wk = jnp.exp(g - m_new[..., None])
        decay = jnp.exp(b_last + m - m_new)
        kw = kc * wk[..., None]
        C_new = decay[..., None, None] * C + jnp.einsum('bhsd,bhse->bhde', kw, vc)
        n_new = decay[..., None] * n + jnp.sum(kw, axis=2)
        return (C_new, n_new, m_new), h

    init = (jnp.zeros((B, H, Dh, Dh), jnp.float32), jnp.zeros((B, H, Dh), jnp.float32),
            jnp.zeros((B, H), jnp.float32))
    _, hs = lax.scan(step, init, (to_chunks(q), to_chunks(k), to_chunks(v), to_chunks(ig), to_chunks(lf)))
    return jnp.moveaxis(hs, (0, 2), (1, 3)).reshape(B, S, H, Dh)


def lru_combine(e1, e2):
    a1, b1 = e1
    a2, b2 = e2
    return a1 * a2, a2 * b1 + b2


def hybrid_mixer(h, w_in, b_gates, conv_qk, mh_norm_g, lru_conv_w, lru_conv_b, w_lru_a, b_lru_a,
                 w_lru_x, b_lru_x, lru_lambda, lru_norm_g, w_out):
    B, S, _ = h.shape
    proj = h @ w_in
    q_raw, k_raw, v_raw, o_raw, i_raw, f_raw, xr_raw, gr_raw = jnp.split(
        proj, list(np.cumsum(IN_SPLITS)[:-1]), axis=-1)

    qk = jax.nn.silu(causal_dwconv(jnp.concatenate([q_raw, k_raw], axis=-1), conv_qk)).astype(jnp.float32)
    q = qk[..., :M_WIDTH].reshape(B, S, M_HEADS, M_HEAD_DIM)
    k = qk[..., M_WIDTH:].reshape(B, S, M_HEADS, M_HEAD_DIM) * (M_HEAD_DIM ** -0.5)
    v = v_raw.astype(jnp.float32).reshape(B, S, M_HEADS, M_HEAD_DIM)
    ig = i_raw.astype(jnp.float32) + b_gates[:M_HEADS]
    lf = jax.nn.log_sigmoid(f_raw.astype(jnp.float32) + b_gates[M_HEADS:])
    hm = mlstm_chunkwise(q, k, v, ig, lf)
    hm = hm * lax.rsqrt(jnp.mean(hm * hm, axis=-1, keepdims=True) + EPS)
    ym = hm.reshape(B, S, M_WIDTH) * mh_norm_g * jax.nn.sigmoid(o_raw.astype(jnp.float32))

    xr = (causal_dwconv(xr_raw, lru_conv_w) + lru_conv_b).astype(jnp.float32)
    xb = xr.reshape(B, S, R_BLOCKS, R_BLOCK_DIM)
    r_gate = jax.nn.sigmoid(jnp.einsum('bsnc,ncd->bsnd', xb, w_lru_a).reshape(B, S, R_WIDTH) + b_lru_a)
    i_gate = jax.nn.sigmoid(jnp.einsum('bsnc,ncd->bsnd', xb, w_lru_x).reshape(B, S, R_WIDTH) + b_lru_x)
    log_a = LRU_C * r_gate * jax.nn.log_sigmoid(lru_lambda.astype(jnp.float32))
    a = jnp.exp(log_a)
    u = jnp.sqrt(-jnp.expm1(2.0 * log_a)) * (i_gate * xr)
    _, hr = lax.associative_scan(lru_combine, (a, u), axis=1)
    yr = hr * jax.nn.gelu(gr_raw.astype(jnp.float32))
    yr = yr.reshape(B, S, R_BLOCKS, R_BLOCK_DIM)
    yr = (yr * lax.rsqrt(jnp.mean(yr * yr, axis=-1, keepdims=True) + EPS)).reshape(B, S, R_WIDTH) * lru_norm_g

    return jnp.concatenate([ym, yr], axis=-1).astype(h.dtype) @ w_out


def hier_moe(xt, w_group, b_group, w_router, b_router, w_e_gate, w_e_up, w_e_down):
    n_tok, d = xt.shape
    xf = xt.astype(jnp.float32)
    p_group = jax.nn.softmax(xf @ w_group + b_group, axis=-1)
    pg_sel, g_sel = lax.top_k(p_group, 1)
    e_logits = (xf @ w_router + b_router).reshape(n_tok, N_GROUPS, EXPERTS_PER_GROUP)
    idx = jnp.broadcast_to(g_sel[:, :, None], (n_tok, 1, EXPERTS_PER_GROUP))
    e_logits = jnp.take_along_axis(e_logits, idx, axis=1)[:, 0]
    pe_sel, e_sel = lax.top_k(jax.nn.softmax(e_logits, axis=-1), TOP_K)
    gate_w = pg_sel * pe_sel / jnp.sum(pe_sel, axis=-1, keepdims=True)
    expert_id = g_sel * EXPERTS_PER_GROUP + e_sel

    m = n_tok * TOP_K
    eid = expert_id.reshape(m)
    tok = jnp.repeat(jnp.arange(n_tok, dtype=jnp.int32), TOP_K)
    wts = gate_w.reshape(m)
    order = jnp.argsort(eid)
    eid_s, tok_s, w_s = eid[order], tok[order], wts[order]
    counts = jnp.bincount(eid, length=N_EXPERTS)
    starts = jnp.cumsum(counts) - counts
    padded = (counts + MOE_BLOCK - 1) // MOE_BLOCK * MOE_BLOCK
    pend = jnp.cumsum(padded)
    pstart = pend - padded
    dest = pstart[eid_s] + jnp.arange(m, dtype=jnp.int32) - starts[eid_s]
    n_slots = -(-m // MOE_BLOCK) * MOE_BLOCK + N_EXPERTS * MOE_BLOCK
    n_blocks = n_slots // MOE_BLOCK
    slot_tok = jnp.zeros((n_slots,), jnp.int32).at[dest].set(tok_s)
    slot_w = jnp.zeros((n_slots,), jnp.float32).at[dest].set(w_s)
    block_e = jnp.minimum(
        jnp.searchsorted(pend, jnp.arange(n_blocks, dtype=pend.dtype) * MOE_BLOCK, side='right'),
        N_EXPERTS - 1)

    def expert_block(args):
        toks, wb, e = args
        xb = xt[toks]
        hb = jax.nn.silu(xb @ w_e_gate[e]) * (xb @ w_e_up[e])
        return (hb @ w_e_down[e]).astype(jnp.float32) * wb[:, None]

    yb = lax.map(expert_block, (slot_tok.reshape(n_blocks, MOE_BLOCK),
                                slot_w.reshape(n_blocks, MOE_BLOCK), block_e))
    return jnp.zeros((n_tok, d), jnp.float32).at[slot_tok].add(yb.reshape(n_slots, d))


def setup_inputs(seed: int = 0) -> dict:
    key = jax.random.key(seed)
    ks = jax.random.split(key, 32)
    L, D, H = DEPTH, D_MODEL, M_HEADS
    nrm = lambda k, shape, s: jax.random.normal(k, shape, jnp.float32) * s
    u = jax.random.uniform(ks[14], (L, R_WIDTH), jnp.float32, minval=0.9, maxval=0.999)
    b_gates = jnp.concatenate([
        nrm(ks[4], (L, H), 0.1),
        jnp.broadcast_to(jnp.linspace(3.0, 6.0, H, dtype=jnp.float32), (L, H)) + nrm(ks[5], (L, H), 0.1)], axis=-1)
    return {
        'x': nrm(ks[0], (BATCH, SEQ, D), 1.0),
        'c': nrm(ks[1], (BATCH, D), 1.0),
        'w_ada': nrm(ks[2], (L, D, 6 * D), D ** -0.5),
        'b_ada': nrm(ks[3], (L, 6 * D), 0.02),
        'w_in': nrm(ks[6], (L, D, IN_WIDTH), D ** -0.5),
        'b_gates': b_gates,
        'conv_qk': nrm(ks[7], (L, CONV_WIDTH, 2 * M_WIDTH), CONV_WIDTH ** -0.5),
        'mh_norm_g': 1.0 + nrm(ks[8], (L, M_WIDTH), 0.02),
        'lru_conv_w': nrm(ks[9], (L, CONV_WIDTH, R_WIDTH), CONV_WIDTH ** -0.5),
        'lru_conv_b': nrm(ks[10], (L, R_WIDTH), 0.02),
        'w_lru_a': nrm(ks[11], (L, R_BLOCKS, R_BLOCK_DIM, R_BLOCK_DIM), R_BLOCK_DIM ** -0.5),
        'b_lru_a': nrm(ks[12], (L, R_WIDTH), 0.02),
        'w_lru_x': nrm(ks[13], (L, R_BLOCKS, R_BLOCK_DIM, R_BLOCK_DIM), R_BLOCK_DIM ** -0.5),
        'b_lru_x': nrm(ks[15], (L, R_WIDTH), 0.02),
        'lru_lambda': jnp.log(u) - jnp.log1p(-u),
        'lru_norm_g': 1.0 + nrm(ks[16], (L, R_WIDTH), 0.02),
        'w_out': nrm(ks[17], (L, D, D), D ** -0.5),
        'w_group': nrm(ks[18], (L, D, N_GROUPS), D ** -0.5),
        'b_group': nrm(ks[19], (L, N_GROUPS), 0.01),
        'w_router': nrm(ks[20], (L, D, N_EXPERTS), D ** -0.5),
        'b_router': nrm(ks[21], (L, N_EXPERTS), 0.01),
        'w_e_gate': nrm(ks[22], (L, N_EXPERTS, D, D_EXPERT), D ** -0.5),
        'w_e_up': nrm(ks[23], (L, N_EXPERTS, D, D_EXPERT), D ** -0.5),
        'w_e_down': nrm(ks[24], (L, N_EXPERTS, D_EXPERT, D), D_EXPERT ** -0.5),
        'final_g': 1.0 + nrm(ks[25], (D,), 0.02),
    }


def reference(x, c, w_ada, b_ada, w_in, b_gates, conv_qk, mh_norm_g, lru_conv_w, lru_conv_b,
              w_lru_a, b_lru_a, w_lru_x, b_lru_x, lru_lambda, lru_norm_g, w_out, w_group, b_group,
              w_router, b_router, w_e_gate, w_e_up, w_e_down, final_g):
    B, S, D = x.shape
    for l in range(DEPTH):
        mod = jax.nn.silu(c) @ w_ada[l] + b_ada[l]
        sh1, sc1, g1, sh2, sc2, g2 = jnp.split(mod, 6, axis=-1)
        hn = rmsnorm(x) * (1.0 + sc1[:, None]) + sh1[:, None]
        mix = hybrid_mixer(hn, w_in[l], b_gates[l], conv_qk[l], mh_norm_g[l], lru_conv_w[l], lru_conv_b[l],
                           w_lru_a[l], b_lru_a[l], w_lru_x[l], b_lru_x[l], lru_lambda[l], lru_norm_g[l], w_out[l])
        x = x + (g1[:, None] * mix).astype(x.dtype)
        hn = rmsnorm(x) * (1.0 + sc2[:, None]) + sh2[:, None]
        y = hier_moe(hn.reshape(B * S, D), w_group[l], b_group[l], w_router[l], b_router[l],
                     w_e_gate[l], w_e_up[l], w_e_down[l]).reshape(B, S, D)
        x = x + (g2[:, None] * y).astype(x.dtype)
    return rmsnorm(x) * final_g
```

```python
import os
from contextlib import ExitStack
import numpy as np
import concourse.bass as bass
import concourse.mybir as mybir
from concourse.bass_utils import run_bass_kernel_spmd

F32 = mybir.dt.float32
BF16 = mybir.dt.bfloat16
ALU = mybir.AluOpType
AF = mybir.ActivationFunctionType
ENGS = ["tensor", "vector", "scalar", "gpsimd", "sync"]

D = 2048
S = 8192
NE = 32
DE = 512
EPS = 1e-6
NT_A = int(os.environ.get("MK_NT_A", "16"))
NEXP = int(os.environ.get("MK_NEXP", "32"))
DEBUG = os.environ.get("MK_DEBUG", "0") == "1"
STOP = int(os.environ.get("MK_STOP", "0"))


HALT = [False]


def stage(k):
    if STOP == k:
        HALT[0] = True
RGROUPS = [[0, 1, 2, 3]] if os.environ.get("MK_SIM4", "0") == "1" else [[0, 1, 2, 3], [4, 5, 6, 7]]


class R:
    __slots__ = ("name", "w", "rs")

    def __init__(self, name=""):
        self.name = name
        self.w = None
        self.rs = {}


class Op:
    __slots__ = ("eng", "emit", "deps", "is_dma", "signal", "sem", "semval", "idx", "inc", "pool")


class Prog:
    def __init__(self):
        self.ops = []

    def add(self, eng, emit, reads=(), writes=(), dma=False):
        if HALT[0]:
            return Op()
        op = Op()
        op.eng, op.emit, op.is_dma = eng, emit, dma
        op.signal, op.sem, op.semval = False, None, 0
        op.idx = len(self.ops)
        op.inc, op.pool = 16, eng
        deps = set()
        for r in reads:
            if r.w is not None:
                deps.add(r.w)
        for r in writes:
            if r.w is not None:
                deps.add(r.w)
            deps.update(r.rs.values())
        deps.discard(op.idx)
        op.deps = deps
        for r in reads:
            r.rs[("dma", op.idx) if dma else eng] = op.idx
        for r in writes:
            r.w = op.idx
            r.rs = {}
        self.ops.append(op)
        return op

    def barrier(self):
        if HALT[0]:
            return
        last = {}
        dmas = set()
        for op in self.ops:
            if op.is_dma:
                dmas.add(op.idx)
            elif op.emit is not None:
                last[op.eng] = op.idx
        for e in ENGS:
            op = self.add(e, None)
            op.deps = set(last.values()) | dmas

    def finalize(self, sems, dma_sems):
        ops = self.ops
        for op in ops:
            for d in op.deps:
                p = ops[d]
                if p.is_dma or p.emit is None:
                    continue
                if p.eng == op.eng and p.eng == "tensor" and not op.is_dma and op.emit is not None:
                    continue
                p.signal = True
        cnt = {e: 0 for e in ENGS}
        dcnt = {e: [0] * len(dma_sems[e]) for e in dma_sems}
        drr = {e: 0 for e in dma_sems}
        pre_wait = {}
        for op in ops:
            if op.is_dma:
                pl = op.pool
                k = drr[pl] % len(dma_sems[pl])
                drr[pl] += 1
                if dcnt[pl][k] > 0:
                    pre_wait[op.idx] = (dma_sems[pl][k], dcnt[pl][k])
                dcnt[pl][k] += op.inc
                op.sem = dma_sems[pl][k]
                op.semval = dcnt[pl][k]
            elif op.signal:
                cnt[op.eng] += 1
                op.sem = sems[op.eng]
                op.semval = cnt[op.eng]
        streams = {e: [] for e in ENGS}
        for op in ops:
            streams[op.eng].append(op)
        self.counts = cnt

        def run_stream(engname, eng):
            waited = {}
            for op in streams[engname]:
                need = {}
                for d in op.deps:
                    p = ops[d]
                    if p.sem is None:
                        continue
                    if p.eng == engname and engname == "tensor" and not p.is_dma and not op.is_dma and op.emit is not None:
                        continue
                    key = id(p.sem)
                    if waited.get(key, 0) >= p.semval:
                        continue
                    if key not in need or need[key][1] < p.semval:
                        need[key] = (p.sem, p.semval)
                if op.idx in pre_wait:
                    s, v = pre_wait[op.idx]
                    key = id(s)
                    if waited.get(key, 0) < v and (key not in need or need[key][1] < v):
                        need[key] = (s, v)
                for key, (s, v) in need.items():
                    eng.wait_ge(s, v)
                    waited[key] = v
                if op.emit is None:
                    continue
                inst = op.emit(eng)
                if op.is_dma:
                    inst.then_inc(op.sem, op.inc)
                elif op.signal:
                    inst.then_inc(op.sem, 1)

        return run_stream


def build_nc():
    nc = bass.Bass("TRN2", target_bir_lowering=False)

    def din(name, shape, dt=F32):
        return nc.dram_tensor(name, list(shape), dt, kind="ExternalInput").ap()

    x_full = din("x_full", [S, D])
    x_seg = din("x_seg", [2048, D])
    c_col = din("c_col", [128, 16])
    w_ada = din("w_ada", [D, 6 * D])
    bada_c = din("bada_c", [128, 96])
    win = din("win", [D, 1792])
    convw = din("convw", [128, 24])
    vecs = din("vecs", [128, 16])
    wlru = din("wlru", [128, 4 * 128])
    wout = din("wout", [D, D])
    wrt = din("wrt", [D, 36])
    brt = din("brt", [128, 36])
    NOBIG = os.environ.get("MK_NOBIG", "0") == "1"
    weg2 = weu2 = wed2 = None
    if not NOBIG:
        weg2 = din("weg", [NE * 128, 8192])
        weu2 = din("weu", [NE * 128, 8192])
        wed2 = din("wed", [NE * 128, 8192])
    fing_bc = din("fing_bc", [128, D])
    consts2 = din("consts2", [128, 128])
    fing = din("fing", [128, 16])
    consts = din("consts", [128, 256])
    segmask = din("segmask", [128, 4])
    out = nc.dram_tensor("out", [2048, D], F32, kind="ExternalOutput").ap()
    cin = [nc.dram_tensor(f"cin{s}", [512, 512], BF16, kind="Internal").ap() for s in range(16)]
    cout = [nc.dram_tensor(f"cout{s}", [2048, 512], BF16, kind="Internal").ap() for s in range(16)]
    hn2_d = nc.dram_tensor("hn2_d", [2048, D], BF16, kind="Internal").ap()
    x1_d = nc.dram_tensor("x1_d", [2048, D], F32, kind="Internal").ap()
    sinfo_d = nc.dram_tensor("sinfo_d", [8192, 2], F32, kind="Internal").ap()
    yb_d = nc.dram_tensor("yb_d", [8192, D], F32, kind="Internal").ap()
    r_hn2d, r_x1d, r_sinfo, r_sinfo2, r_ybd = R(), R(), R(), R(), R()
    NBLK_RUN = int(os.environ.get("MK_NBLK", "64"))
    dbg = {}
    if DEBUG:
        for nm, shp, dt in [("d_mod", [128, 96], F32), ("d_hnT", [128, 16 * 512], BF16), ("d_y", [128, 4 * 512], BF16),
                            ("d_x1T", [128, 16 * 512], F32), ("d_hn2T", [128, 16 * 512], BF16), ("d_cw", [128, 4 * 32], F32),
                            ("d_q", [128, 2 * 512], BF16), ("d_k", [128, 2 * 512], BF16), ("d_hm", [128, 2 * 512], F32),
                            ("d_x2T", [128, 16 * 512], F32), ("d_x1", [128, D], F32), ("d_hn2", [128, D], BF16),
                            ("d_dst", [128, 32], F32), ("d_ebk", [128, 64], F32)]:
            dbg[nm] = nc.dram_tensor(nm, shp, dt, kind="ExternalOutput").ap()

    P = Prog()
    V = lambda f, rd, wr: P.add("vector", f, rd, wr)
    A = lambda f, rd, wr: P.add("scalar", f, rd, wr)
    G = lambda f, rd, wr: P.add("gpsimd", f, rd, wr)

    def MM(o, l, r, st, sp, rd, wr):
        return P.add("tensor", lambda e: e.matmul(o, l, r, start=st, stop=sp), rd, wr)

    def TR(o, i, ident, rd, wr):
        return P.add("tensor", lambda e: e.transpose(o, i, ident), rd, wr)

    def DMA(eng, o, i, rd, wr):
        return P.add(eng, lambda e: e.dma_start(out=o, in_=i), rd, wr, dma=True)

    def act(o, i, f, rd, wr, **kw):
        return A(lambda e: e.activation(out=o, in_=i, func=f, **kw), rd, wr)

    def tt(o, a, b, op, rd, wr, eng="vector"):
        return P.add(eng, lambda e: e.tensor_tensor(out=o, in0=a, in1=b, op=op), rd, wr)

    def ts(o, a, s1, s2, op0, op1, rd, wr, eng="vector"):
        if op1 is None:
            return P.add(eng, lambda e: e.tensor_scalar(out=o, in0=a, scalar1=s1, scalar2=None, op0=op0), rd, wr)
        return P.add(eng, lambda e: e.tensor_scalar(out=o, in0=a, scalar1=s1, scalar2=s2, op0=op0, op1=op1), rd, wr)

    def stt(o, a, s, b, op0, op1, rd, wr):
        return V(lambda e: e.scalar_tensor_tensor(out=o, in0=a, scalar=s, in1=b, op0=op0, op1=op1), rd, wr)

    with ExitStack() as es0:
        sems = {e: es0.enter_context(nc.semaphore("s_" + e)) for e in ENGS}
        dsems = {e: [es0.enter_context(nc.semaphore(f"d_{e}{i}")) for i in range(8)] for e in ["sync", "gpsimd"]}
        dsems["cc"] = [es0.enter_context(nc.semaphore("cc_sem"))]

        def SB(es, name, shape, dt=F32):
            return es.enter_context(nc.sbuf_tensor(name, list(shape), dt))

        cst = SB(es0, "cst", [128, 256]); r_cst = R()
        cst2 = SB(es0, "cst2", [128, 128])
        ident_f = cst[:, 0:128]
        tri = cst[:, 128:256]
        ident_b = SB(es0, "ident_b", [128, 128], BF16)
        ones_f = SB(es0, "ones_f", [128, 128])
        ones_b = SB(es0, "ones_b", [128, 128], BF16)
        c256_b = SB(es0, "c256_b", [128, 128], BF16)
        c128_b = SB(es0, "c128_b", [128, 128], BF16)
        c2048_b = SB(es0, "c2048_b", [128, 128], BF16)
        modc = SB(es0, "modc", [128, 96]); r_modc = R()
        modp = SB(es0, "modp", [128, 32]); r_modp = R()
        vec = SB(es0, "vec", [128, 16]); r_vec = R()
        lsv = SB(es0, "lsv", [128, 8]); r_lsv = R()
        cwv = SB(es0, "cwv", [128, 24]); r_cwv = R()
        fg = SB(es0, "fg", [128, 16]); r_fg = R()
        smk = SB(es0, "smk", [128, 4]); r_smk = R()
        brt_t = SB(es0, "brt_t", [128, 36]); r_brt = R()
        wrt_b = SB(es0, "wrt_b", [128, 16, 36], BF16); r_wrt = R()
        ps_trs = [es0.enter_context(nc.psum_tensor(f"ps_tr{i}", [128, 1024], BF16)) for i in range(2)]
        r_ptr = [R(), R()]
        psb = [es0.enter_context(nc.psum_tensor(f"psb{i}", [128, 512], F32)) for i in range(6)]
        r_psb = [R() for _ in range(6)]
        gen_rr = [0]

        def genbank(pool):
            i = pool[gen_rr[0] % len(pool)]
            gen_rr[0] += 1
            return psb[i], r_psb[i]

        DMA("sync", cst[:], consts, [], [r_cst])
        DMA("sync", cst2[:], consts2, [], [r_cst])
        DMA("sync", vec[:], vecs, [], [r_vec])
        DMA("sync", cwv[:], convw, [], [r_cwv])
        DMA("sync", fg[:], fing, [], [r_fg])
        DMA("sync", smk[:], segmask, [], [r_smk])
        DMA("sync", brt_t[:], brt, [], [r_brt])
        DMA("sync", modc[:], bada_c, [], [r_modc])
        DMA("gpsimd", wrt_b[:], wrt.rearrange("(kc p) n -> p kc n", p=128), [], [r_wrt])
        r_k = R()
        V(lambda e: e.memset(ones_f[:], 1.0), [], [r_k])
        V(lambda e: e.memset(ones_b[:], 1.0), [], [r_k])
        V(lambda e: e.memset(c256_b[:], 1.0 / 256), [], [r_k])
        V(lambda e: e.memset(c128_b[:], 1.0 / 128), [], [r_k])
        V(lambda e: e.memset(c2048_b[:], 1.0 / 2048), [], [r_k])
        V(lambda e: e.tensor_copy(out=ident_b[:], in_=ident_f), [r_cst], [r_k])

        try:
            with ExitStack() as esA:
                POOL_A = [0, 1, 2]
                B_S, B_N, B_C0 = 3, 4, 5
                win_b = SB(esA, "win_b", [128, 16, 1792], BF16); r_win = R()
                for kc in range(16):
                    DMA("gpsimd", win_b[:, kc, :], win[kc * 128:(kc + 1) * 128, :], [], [r_win])
                esL = esA.enter_context(ExitStack())
                ccol = SB(esL, "ccol", [128, 16]); r_cc = R()
                ccol_b = SB(esL, "ccol_b", [128, 16], BF16)
                DMA("sync", ccol[:], c_col, [], [r_cc])
                act(ccol_b[:], ccol[:], AF.Silu, [r_cc], [r_cc])
                NGW = 256
                wa = [SB(esL, f"wa{i}", [128, 16, NGW], BF16) for i in range(2)]
                r_wa = [R(), R()]
                rowb = SB(esL, "rowb", [1, NGW]); r_row = R()
                for ng in range(6 * D // NGW):
                    bi = ng % 2
                    for h in range(2):
                        DMA("gpsimd", wa[bi][:, h * 8:(h + 1) * 8, :],
                            w_ada[h * 1024:(h + 1) * 1024, ng * NGW:(ng + 1) * NGW].rearrange("(kc p) n -> p kc n", p=128),
                            [], [r_wa[bi]])
                    pb, rp = genbank(POOL_A)
                    for kc in range(16):
                        MM(pb[0:1, 0:NGW], ccol_b[:, kc:kc + 1], wa[bi][:, kc, :], kc == 0, kc == 15, [r_cc, r_wa[bi]], [rp])
                    act(rowb[:], pb[0:1, 0:NGW], AF.Copy, [rp], [r_row])
                    pb2, rp2 = genbank(POOL_A)
                    nq = NGW // 128
                    for q in range(nq):
                        MM(pb2[:, q:q + 1], rowb[0:1, q * 128:(q + 1) * 128], ones_f[0:1, 0:1], True, True, [r_row, r_k], [rp2])
                    c0 = ng * nq
                    tt(modc[:, c0:c0 + nq], modc[:, c0:c0 + nq], pb2[:, 0:nq], ALU.add, [rp2, r_modc], [r_modc])
                ts(modp[:, 0:16], modc[:, 16:32], 1.0, None, ALU.add, None, [r_modc], [r_modp])
                ts(modp[:, 16:32], modc[:, 64:80], 1.0, None, ALU.add, None, [r_modc], [r_modp])
                if DEBUG:
                    DMA("sync", dbg["d_mod"], modc[:], [r_modc], [])
                P.barrier()
                esL.close()
                stage(1)
                lt = SB(esA, "lt", [128, 2])
                act(lt[:], vec[:, 6:8], AF.Exp, [r_vec], [r_lsv], scale=-1.0)
                ts(lt[:], lt[:], 1.0, None, ALU.add, None, [r_lsv], [r_lsv])
                act(lt[:], lt[:], AF.Ln, [r_lsv], [r_lsv])
                ts(lsv[:, 0:2], lt[:], -8.0, None, ALU.mult, None, [r_lsv], [r_lsv])
                ts(lsv[:, 2:4], lt[:], -16.0, None, ALU.mult, None, [r_lsv], [r_lsv])
                wl_f = SB(esA, "wl_f", [128, 512]); r_wl = R()
                wl_b = SB(esA, "wl_b", [128, 512], BF16)
                DMA("sync", wl_f[:], wlru, [], [r_wl])
                V(lambda e: e.tensor_copy(out=wl_b[:], in_=wl_f[:]), [r_wl], [r_wl])

                xt = [SB(esA, f"xt{i}", [128, D]) for i in range(2)]; r_xt = [R() for _ in range(2)]
                junk = SB(esA, "junk", [128, D], BF16); r_junk = R()
                xs = SB(esA, "xs", [128, 4, D], BF16); r_xs = [R() for _ in range(4)]
                ssq = SB(esA, "ssq", [128, 8]); r_ssq = R()
                hnT = SB(esA, "hnT", [128, 16, 512], BF16); r_hnT = R()
                raw = SB(esA, "raw", [128, 515]); r_raw = R()
                halo = SB(esA, "halo", [128, 6, 3]); r_halo = [R() for _ in range(6)]
                acc = SB(esA, "acc", [128, 512]); r_acc = R()
                tmpA = SB(esA, "tmpA", [128, 512]); r_tmpA = R()
                qT = SB(esA, "qT", [128, 2, 512], BF16); r_q = R()
                kT = SB(esA, "kT", [128, 2, 512], BF16); r_kk = R()
                q2T = SB(esA, "q2T", [128, 2, 512], BF16); r_q2 = R()
                og = SB(esA, "og", [128, 2, 512]); r_og = R()
                xrf = SB(esA, "xrf", [128, 2, 512]); r_xrf = R()
                xrb = SB(esA, "xrb", [128, 2, 512], BF16)
                gl = SB(esA, "gl", [128, 2, 512]); r_gl = R()
                ibc = SB(esA, "ibc", [128, 512]); r_ibc = R()
                nlf = SB(esA, "nlf", [128, 512]); r_nlf = R()
                nbb = SB(esA, "nbb", [128, 512]); r_nbb = R()
                ebb = SB(esA, "ebb", [128, 512]); r_ebb = R()
                gtk = SB(esA, "gtk", [128, 32]); r_gtk = R()
                zt = SB(esA, "zt", [128, 512]); r_zt = R()
                dm = SB(esA, "dm", [128, 512]); r_dm = R()
                tri4 = SB(esA, "tri4", [128, 512]); r_tri4 = R()
                PT = SB(esA, "PT", [128, 512], BF16); r_PT = R()
                vext = SB(esA, "vext", [128, 4, 258], BF16); r_vext = [R() for _ in range(4)]
                kw = SB(esA, "kw", [128, 4, 256], BF16); r_kw = [R() for _ in range(4)]
                Cf = SB(esA, "Cf", [128, 2, 257]); r_Cf = R()
                Cb = SB(esA, "Cb", [128, 2, 256], BF16); r_Cb = R()
                nbc = SB(esA, "nbc", [128, 2, 128], BF16); r_nbc = R()
                dab = SB(esA, "dab", [128, 128]); r_dab = R()
                hmT = SB(esA, "hmT", [128, 2, 512]); r_hm = R()
                sqb = SB(esA, "sqb", [128, 2, 512], BF16); r_sq = R()
                rsb = SB(esA, "rsb", [128, 512]); r_rs = R()
                lr = SB(esA, "lr", [128, 512]); r_lr = R()
                li = SB(esA, "li", [128, 512]); r_li = R()
                la, r_la = zt, r_zt
                lm, r_lm = dm, r_dm
                lh = SB(esA, "lh", [128, 512]); r_lh = R()
                hprev = SB(esA, "hprev", [128, 2]); r_hp = R()
                ytile = [SB(esA, f"ytile{i}", [128, 4, 512], BF16) for i in range(2)]; r_yt = [R(), R()]
                r_cin = [R() for _ in range(16)]
                r_cout = [R() for _ in range(16)]

                V(lambda e: e.memset(halo[:], 0.0), [], r_halo)
                V(lambda e: e.memset(Cf[:], 0.0), [], [r_Cf])
                V(lambda e: e.memset(Cb[:], 0.0), [], [r_Cb])
                V(lambda e: e.memset(nbc[:], 0.0), [], [r_nbc])
                V(lambda e: e.memset(hprev[:], 0.0), [], [r_hp])
                V(lambda e: e.memset(vext[:], 1.0), [], r_vext)
                for c in range(4):
                    V(lambda e, c=c: e.tensor_copy(out=tri4[:, c * 128:(c + 1) * 128], in_=tri), [r_cst], [r_tri4])

                xrr = [0]
                for it in range(NT_A):
                    for s in range(4):
                        xb = xrr[0] % 2
                        xrr[0] += 1
                        row0 = (it * 4 + s) * 128
                        DMA("sync", xt[xb][:], x_full[row0:row0 + 128, :], [], [r_xt[xb]])
                        A(lambda e, xb=xb, s=s: e.activation(out=junk[:], in_=xt[xb][:], func=AF.Square, accum_out=ssq[:, s:s + 1]),
                          [r_xt[xb]], [r_junk, r_ssq])
                        ts(ssq[:, 4 + s:5 + s], ssq[:, s:s + 1], 1.0 / D, EPS, ALU.mult, ALU.add, [r_ssq], [r_ssq])
                        act(ssq[:, 4 + s:5 + s], ssq[:, 4 + s:5 + s], AF.Sqrt, [r_ssq], [r_ssq])
                        V(lambda e, s=s: e.reciprocal(out=ssq[:, 4 + s:5 + s], in_=ssq[:, 4 + s:5 + s]), [r_ssq], [r_ssq])
                        ts(xs[:, s, :], xt[xb][:], ssq[:, 4 + s:5 + s], None, ALU.mult, None, [r_ssq, r_xt[xb]], [r_xs[s]])
                    for dc in range(16):
                        h = dc % 2
                        for s in range(4):
                            TR(ps_trs[h][:, s * 128:(s + 1) * 128], xs[:, s, dc * 128:(dc + 1) * 128], ident_b[:],
                               [r_xs[s], r_k], [r_ptr[h]])
                        act(hnT[:, dc, :], ps_trs[h][:, 0:512], AF.Identity, [r_ptr[h], r_modp, r_modc], [r_hnT],
                            scale=modp[:, dc:dc + 1], bias=modc[:, dc:dc + 1])
                    stage(2)
                    if DEBUG and it == 0:
                        DMA("sync", dbg["d_hnT"], hnT[:].rearrange("p a b -> p (a b)"), [r_hnT], [])
                    yt = ytile[it % 2]
                    r_y = r_yt[it % 2]
                    for blk in range(12):
                        pb, rp = genbank(POOL_A)
                        for kc in range(16):
                            MM(pb[:], win_b[:, kc, blk * 128:(blk + 1) * 128], hnT[:, kc, :], kc == 0, kc == 15, [r_win, r_hnT], [rp])
                        if blk < 4 or 6 <= blk < 8:
                            cb = blk if blk < 4 else blk - 2
                            V(lambda e, cb=cb: e.tensor_copy(out=raw[:, 0:3], in_=halo[:, cb, :]), [r_halo[cb]], [r_raw])
                            act(raw[:, 3:515], pb[:], AF.Copy, [rp], [r_raw])
                            ts(acc[:], raw[:, 3:515], cwv[:, cb * 4 + 3:cb * 4 + 4], None, ALU.mult, None, [r_raw, r_cwv], [r_acc])
                            for j in range(3):
                                stt(acc[:], raw[:, j:j + 512], cwv[:, cb * 4 + j:cb * 4 + j + 1], acc[:], ALU.mult, ALU.add,
                                    [r_raw, r_acc], [r_acc])
                            V(lambda e, cb=cb: e.tensor_copy(out=halo[:, cb, :], in_=raw[:, 512:515]), [r_raw], [r_halo[cb]])
                            if cb < 2:
                                act(qT[:, cb, :], acc[:], AF.Silu, [r_acc], [r_q])
                            elif cb < 4:
                                act(tmpA[:], acc[:], AF.Sigmoid, [r_acc], [r_tmpA])
                                stt(kT[:, cb - 2, :], tmpA[:], 1.0 / 16, acc[:], ALU.mult, ALU.mult, [r_tmpA, r_acc], [r_kk])
                            else:
                                bi = cb - 4
                                act(xrf[:, bi, :], acc[:], AF.Identity, [r_acc, r_vec], [r_xrf], bias=vec[:, 0 + bi:1 + bi])
                                V(lambda e, bi=bi: e.tensor_copy(out=xrb[:, bi, :], in_=xrf[:, bi, :]), [r_xrf], [r_xrf])
                        elif blk < 6:
                            act(og[:, blk - 4, :], pb[:], AF.Sigmoid, [rp], [r_og])
                        elif blk < 10:
                            bi = blk - 8
                            act(gl[:, bi, :], pb[:], AF.Gelu_apprx_tanh, [rp], [r_gl])
                        elif blk == 10:
                            act(ibc[:], pb[:], AF.Identity, [rp, r_vec], [r_ibc], bias=vec[:, 12:13])
                        else:
                            act(nlf[:], pb[:], AF.Identity, [rp, r_vec], [r_nlf], bias=vec[:, 13:14])
                            act(nlf[:], nlf[:], AF.Exp, [r_nlf], [r_nlf], scale=-1.0)
                            ts(nlf[:], nlf[:], 1.0, None, ALU.add, None, [r_nlf], [r_nlf])
                            act(nlf[:], nlf[:], AF.Ln, [r_nlf], [r_nlf])
                    if DEBUG and it == 0:
                        DMA("sync", dbg["d_q"], qT[:].rearrange("p a b -> p (a b)"), [r_q], [])
                        DMA("sync", dbg["d_k"], kT[:].rearrange("p a b -> p (a b)"), [r_kk], [])
                    stage(3)
                    for s in range(4):
                        pb, rp = genbank(POOL_A)
                        for kc in range(16):
                            MM(pb[:, 0:256], hnT[:, kc, s * 128:(s + 1) * 128], win_b[:, kc, 1536:1792], kc == 0, kc == 15,
                               [r_win, r_hnT], [rp])
                        act(vext[:, s, 0:256], pb[:, 0:256], AF.Copy, [rp], [r_vext[s]])
                    for c in range(4):
                        V(lambda e, c=c: e.tensor_tensor_scan(out=nbb[:, c * 128:(c + 1) * 128], data0=ones_f[:],
                                                              data1=nlf[:, c * 128:(c + 1) * 128], initial=0.0,
                                                              op0=ALU.mult, op1=ALU.add), [r_nlf, r_k], [r_nbb])
                    for c in range(4):
                        cs = slice(c * 128, (c + 1) * 128)
                        tt(zt[:, cs], ibc[:, cs], ident_f, ALU.mult, [r_ibc, r_cst], [r_zt])
                        V(lambda e, c=c, cs=cs: e.reduce_sum(out=gtk[:, c:c + 1], in_=zt[:, cs], axis=mybir.AxisListType.X),
                          [r_zt], [r_gtk])
                        tt(dm[:, cs], nbb[:, cs], ident_f, ALU.mult, [r_nbb, r_cst], [r_dm])
                        V(lambda e, c=c, cs=cs: e.reduce_sum(out=gtk[:, 8 + c:9 + c], in_=dm[:, cs], axis=mybir.AxisListType.X),
                          [r_dm], [r_gtk])
                        V(lambda e, c=c: e.tensor_copy(out=gtk[:, 12 + c:13 + c], in_=nbb[:, c * 128 + 127:c * 128 + 128]),
                          [r_nbb], [r_gtk])
                    act(ebb[:], nbb[:], AF.Exp, [r_nbb], [r_ebb], scale=-1.0)
                    for dk in range(2):
                        tt(q2T[:, dk, :], qT[:, dk, :], ebb[:], ALU.mult, [r_q, r_ebb], [r_q2])
                    tt(gtk[:, 24:28], gtk[:, 12:16], gtk[:, 8:12], ALU.subtract, [r_gtk], [r_gtk])
                    tt(gtk[:, 24:28], gtk[:, 0:4], gtk[:, 24:28], ALU.subtract, [r_gtk], [r_gtk])
                    act(gtk[:, 16:20], gtk[:, 24:28], AF.Exp, [r_gtk], [r_gtk])
                    act(gtk[:, 20:24], gtk[:, 12:16], AF.Exp, [r_gtk], [r_gtk], scale=-1.0)
                    stage(4)
                    for c in range(4):
                        cs = slice(c * 128, (c + 1) * 128)
                        for dk in range(2):
                            MM(psb[B_S][:, cs], kT[:, dk, cs], qT[:, dk, cs], dk == 0, dk == 1, [r_kk, r_q], [r_psb[B_S]])
                        ts(zt[:, cs], nbb[:, cs], gtk[:, 8 + c:9 + c], 0.0, ALU.subtract, ALU.max, [r_nbb, r_gtk], [r_zt])
                        act(dm[:, cs], zt[:, cs], AF.Exp, [r_zt, r_gtk], [r_dm], scale=-1.0, bias=gtk[:, c:c + 1])
                    tt(dm[:], dm[:], tri4[:], ALU.mult, [r_dm, r_tri4], [r_dm])
                    tt(PT[:], psb[B_S][:], dm[:], ALU.mult, [r_psb[B_S], r_dm], [r_PT])
                    for c in range(4):
                        cs = slice(c * 128, (c + 1) * 128)
                        h = c % 2
                        for dk in range(2):
                            TR(ps_trs[h][:, dk * 128:(dk + 1) * 128], kT[:, dk, cs], ident_b[:], [r_kk, r_k], [r_ptr[h]])
                        act(kw[:, c, :], ps_trs[h][:, 0:256], AF.Identity, [r_ptr[h], r_gtk], [r_kw[c]], scale=gtk[:, 16 + c:17 + c])
                    stage(5)
                    for c in range(4):
                        cs = slice(c * 128, (c + 1) * 128)
                        pN, rN = psb[B_N], r_psb[B_N]
                        for e_ in range(2):
                            es_ = slice(e_ * 128, (e_ + 1) * 128)
                            MM(pN[:, es_], vext[:, c, es_], PT[:, cs], True, False, [r_vext[c], r_PT], [rN])
                            MM(pN[:, es_], Cb[:, 0, es_], q2T[:, 0, cs], False, False, [r_Cb, r_q2], [rN])
                            MM(pN[:, es_], Cb[:, 1, es_], q2T[:, 1, cs], False, True, [r_Cb, r_q2], [rN])
                        MM(pN[:, 256:384], ones_b[:], PT[:, cs], True, False, [r_k, r_PT], [rN])
                        MM(pN[:, 256:384], nbc[:, 0, :], q2T[:, 0, cs], False, False, [r_nbc, r_q2], [rN])
                        MM(pN[:, 256:384], nbc[:, 1, :], q2T[:, 1, cs], False, True, [r_nbc, r_q2], [rN])
                        act(dab[:], pN[:, 256:384], AF.Abs, [rN], [r_dab])
                        ts(dab[:], dab[:], 1.0, None, ALU.max, None, [r_dab], [r_dab])
                        V(lambda e: e.reciprocal(out=dab[:], in_=dab[:]), [r_dab], [r_dab])
                        for e_ in range(2):
                            tt(hmT[:, e_, cs], pN[:, e_ * 128:(e_ + 1) * 128], dab[:], ALU.mult, [rN, r_dab], [r_hm])
                        for dk in range(2):
                            bk = B_C0
                            MM(psb[bk][:, 0:257], kw[:, c, dk * 128:(dk + 1) * 128], vext[:, c, 0:257], True, True,
                               [r_kw[c], r_vext[c]], [r_psb[bk]])
                            stt(Cf[:, dk, :], Cf[:, dk, :], gtk[:, 20 + c:21 + c], psb[bk][:, 0:257], ALU.mult, ALU.add,
                                [r_Cf, r_gtk, r_psb[bk]], [r_Cf])
                            act(Cb[:, dk, :], Cf[:, dk, 0:256], AF.Copy, [r_Cf], [r_Cb])
                            ts(nbc[:, dk, :], ones_f[:], Cf[:, dk, 256:257], None, ALU.mult, None, [r_Cf, r_k], [r_nbc])
                    if DEBUG and it == 0:
                        DMA("sync", dbg["d_hm"], hmT[:].rearrange("p a b -> p (a b)"), [r_hm], [])
                    stage(6)
                    act(sqb[:], hmT[:], AF.Square, [r_hm], [r_sq])
                    pb, rp = genbank(POOL_A)
                    MM(pb[:], c256_b[:], sqb[:, 0, :], True, False, [r_k, r_sq], [rp])
                    MM(pb[:], c256_b[:], sqb[:, 1, :], False, True, [r_k, r_sq], [rp])
                    ts(rsb[:], pb[:], EPS, None, ALU.add, None, [rp], [r_rs])
                    act(rsb[:], rsb[:], AF.Sqrt, [r_rs], [r_rs])
                    V(lambda e: e.reciprocal(out=rsb[:], in_=rsb[:]), [r_rs], [r_rs])
                    for e_ in range(2):
                        stt(tmpA[:], hmT[:, e_, :], vec[:, 10 + e_:11 + e_], rsb[:], ALU.mult, ALU.mult, [r_hm, r_vec, r_rs], [r_tmpA])
                        tt(yt[:, e_, :], tmpA[:], og[:, e_, :], ALU.mult, [r_tmpA, r_og], [r_y])
                    stage(7)
                    for bi in range(2):
                        pa, rpa = genbank(POOL_A)
                        MM(pa[:], wl_b[:, bi * 128:(bi + 1) * 128], xrb[:, bi, :], True, True, [r_wl, r_xrf], [rpa])
                        px, rpx = genbank(POOL_A)
                        MM(px[:], wl_b[:, (2 + bi) * 128:(3 + bi) * 128], xrb[:, bi, :], True, True, [r_wl, r_xrf], [rpx])
                        act(lr[:], pa[:], AF.Sigmoid, [rpa, r_vec], [r_lr], bias=vec[:, 2 + bi:3 + bi])
                        act(li[:], px[:], AF.Sigmoid, [rpx, r_vec], [r_li], bias=vec[:, 4 + bi:5 + bi])
                        act(la[:], lr[:], AF.Exp, [r_lr, r_lsv], [r_la], scale=lsv[:, bi:bi + 1])
                        act(lm[:], lr[:], AF.Exp, [r_lr, r_lsv], [r_lm], scale=lsv[:, 2 + bi:3 + bi])
                        ts(lm[:], lm[:], -1.0, 1.0, ALU.mult, ALU.add, [r_lm], [r_lm])
                        act(lm[:], lm[:], AF.Sqrt, [r_lm], [r_lm])
                        tt(lm[:], lm[:], li[:], ALU.mult, [r_lm, r_li], [r_lm])
                        tt(lm[:], lm[:], xrf[:, bi, :], ALU.mult, [r_lm, r_xrf], [r_lm])
                        V(lambda e, bi=bi: e.tensor_tensor_scan(out=lh[:], data0=la[:], data1=lm[:], initial=hprev[:, bi:bi + 1],
                                                                op0=ALU.mult, op1=ALU.add), [r_la, r_lm, r_hp], [r_lh])
                        V(lambda e, bi=bi: e.tensor_copy(out=hprev[:, bi:bi + 1], in_=lh[:, 511:512]), [r_lh], [r_hp])
                        tt(lh[:], lh[:], gl[:, bi, :], ALU.mult, [r_lh, r_gl], [r_lh])
                        act(sqb[:, 0, :], lh[:], AF.Square, [r_lh], [r_sq])
                        pb, rp = genbank(POOL_A)
                        MM(pb[:], c128_b[:], sqb[:, 0, :], True, True, [r_k, r_sq], [rp])
                        ts(rsb[:], pb[:], EPS, None, ALU.add, None, [rp], [r_rs])
                        act(rsb[:], rsb[:], AF.Sqrt, [r_rs], [r_rs])
                        V(lambda e: e.reciprocal(out=rsb[:], in_=rsb[:]), [r_rs], [r_rs])
                        stt(yt[:, 2 + bi, :], lh[:], vec[:, 8 + bi:9 + bi], rsb[:], ALU.mult, ALU.mult, [r_lh, r_vec, r_rs], [r_y])
                    stage(8)
                    DMA("sync", cin[it].rearrange("(blk p) t -> p blk t", p=128), yt[:], [r_y], [r_cin[it]])
                    if DEBUG and it == 0:
                        DMA("sync", dbg["d_y"], yt[:].rearrange("p a b -> p (a b)"), [r_y], [])
                    cc = P.add("gpsimd", lambda e, it=it: e.collective_compute(
                        "AllGather", ALU.bypass, replica_groups=RGROUPS,
                        ins=[cin[it].opt()], outs=[cout[it].opt()]), [r_cin[it]], [r_cout[it]], dma=True)
                    cc.inc, cc.pool = 1, "cc"
                    stage(9)
                P.barrier()

            with ExitStack() as esC:
                POOL_C = [0, 1, 2, 3, 4, 5]
                I32 = mybir.dt.int32
                NSUB = 16
                NBLK = 64
                bc_g1 = SB(esC, "bc_g1", [128, D]); bc_s2 = SB(esC, "bc_s2", [128, D]); bc_h2 = SB(esC, "bc_h2", [128, D])
                bc_g2 = SB(esC, "bc_g2", [128, D]); bc_fg = SB(esC, "bc_fg", [128, D]); r_bc = R()
                oh1s = SB(esC, "oh1s", [128, NSUB, 32]); oh2s = SB(esC, "oh2s", [128, NSUB, 32]); r_ohs = R()
                w12 = SB(esC, "w12", [128, NSUB, 2]); r_w12 = R()
                rank = SB(esC, "rank", [128, NSUB, 32]); r_rank = R()
                desti = SB(esC, "desti", [128, 2 * NSUB], I32); r_desti = R()
                widx = SB(esC, "widx", [128, NBLK], I32); r_widx = R()
                striu = SB(esC, "striu", [128, 128]); r_striu = R()
                diag = [SB(esC, f"diag{i}", [128, 128]) for i in range(2)]; r_diag = [R(), R()]
                tt(striu[:], tri, ident_f, ALU.subtract, [r_cst], [r_striu])
                DMA("sync", bc_fg[:], fing_bc, [], [r_bc])

                dgh = [SB(esC, f"dgh{i}", [128, 128], BF16) for i in range(2)]
                dgl = [SB(esC, f"dgl{i}", [128, 128], BF16) for i in range(2)]

                def bcast(dst, col0):
                    for dg in range(4):
                        pb, rp = genbank(POOL_C)
                        for q in range(4):
                            ci = dg * 4 + q
                            dgt, rdg = diag[ci % 2], r_diag[ci % 2]
                            hi_, lo_ = dgh[ci % 2], dgl[ci % 2]
                            src = modp if col0 < 0 else modc
                            c_ = (16 + ci) if col0 < 0 else (col0 + ci)
                            ts(dgt[:], ident_f, src[:, c_:c_ + 1], None, ALU.mult, None, [r_cst, r_modc, r_modp], [rdg])
                            V(lambda e, hi_=hi_, dgt=dgt: e.tensor_copy(out=hi_[:], in_=dgt[:]), [rdg], [rdg])
                            tt(dgt[:], dgt[:], hi_[:], ALU.subtract, [rdg], [rdg])
                            V(lambda e, lo_=lo_, dgt=dgt: e.tensor_copy(out=lo_[:], in_=dgt[:]), [rdg], [rdg])
                            MM(pb[:, q * 128:(q + 1) * 128], ones_b[:], hi_[:], True, False, [r_k, rdg], [rp])
                            MM(pb[:, q * 128:(q + 1) * 128], ones_b[:], lo_[:], False, True, [r_k, rdg], [rp])
                        act(dst[:, dg * 512:(dg + 1) * 512], pb[:], AF.Copy, [rp], [r_bc])
                bcast(bc_g1, 32)
                bcast(bc_s2, -1)
                bcast(bc_h2, 48)
                bcast(bc_g2, 80)

                with ExitStack() as esC1:
                    xl = [SB(esC1, f"xl{i}", [128, D]) for i in range(2)]; r_xl = [R(), R()]
                    ytl = SB(esC1, "ytl", [128, 16, 512], BF16); r_ytl = R()
                    ytmp = SB(esC1, "ytmp", [128, 16, 512], BF16); r_ytmp = R()
                    wo = [SB(esC1, f"wo{i}", [128, 16, 512], BF16) for i in range(4)]; r_wo = R()
                    junk2 = SB(esC1, "junk2", [128, D], BF16); r_junk2 = R()
                    tmpc = SB(esC1, "tmpc", [128, D]); r_tmpc = R()
                    hnb = [SB(esC1, f"hnb{i}", [128, D], BF16) for i in range(2)]; r_hnb = [R(), R()]
                    hnp = [SB(esC1, f"hnp{i}", [128, D], BF16) for i in range(2)]; r_hnp = [R(), R()]
                    hT16 = SB(esC1, "hT16", [128, 16, 128], BF16); r_hT16 = R()
                    ss2 = SB(esC1, "ss2", [128, 4]); r_ss2 = R()
                    lg = SB(esC1, "lg", [128, 36]); r_lg = R()
                    rt = SB(esC1, "rt", [128, 64]); r_rt = R()
                    for dgp in range(4):
                        for h in range(2):
                            DMA("gpsimd", wo[dgp][:, h * 8:(h + 1) * 8, :],
                                wout[h * 1024:(h + 1) * 1024, dgp * 512:(dgp + 1) * 512].rearrange("(kc p) n -> p kc n", p=128),
                                [], [r_wo])
                    for tt_ in range(4):
                        t0 = tt_ * 512
                        for sgm in range(min(4, NT_A // 4)):
                            src = cout[4 * sgm + tt_].rearrange("(kc p) t -> p kc t", p=128)
                            r_src = r_cout[4 * sgm + tt_]
                            DMA("sync", ytmp[:], src, [r_src], [r_ytmp])
                            if sgm == 0:
                                ts(ytl[:], ytmp[:], smk[:, 0:1], None, ALU.mult, None, [r_ytmp, r_smk], [r_ytl])
                            else:
                                stt(ytl[:].rearrange("p a b -> p (a b)"), ytmp[:].rearrange("p a b -> p (a b)"), smk[:, sgm:sgm + 1],
                                    ytl[:].rearrange("p a b -> p (a b)"), ALU.mult, ALU.add, [r_ytmp, r_smk, r_ytl], [r_ytl])
                        for s in range(4):
                            si = tt_ * 4 + s
                            xb = si % 2
                            x1 = xl[xb]
                            rx1 = r_xl[xb]
                            DMA("sync", x1[:], x_seg[t0 + s * 128:t0 + (s + 1) * 128, :], [], [rx1])
                            for dg in range(4):
                                pb, rp = genbank(POOL_C)
                                for kc in range(16):
                                    MM(pb[:], ytl[:, kc, s * 128:(s + 1) * 128], wo[dg][:, kc, :], kc == 0, kc == 15, [r_ytl, r_wo], [rp])
                                dsl = slice(dg * 512, (dg + 1) * 512)
                                tt(tmpc[:, dsl], pb[:], bc_g1[:, dsl], ALU.mult, [rp, r_bc], [r_tmpc])
                                tt(x1[:, dsl], x1[:, dsl], tmpc[:, dsl], ALU.add, [r_tmpc, rx1], [rx1])
                            DMA("sync", x1_d[t0 + s * 128:t0 + (s + 1) * 128, :], x1[:], [rx1], [r_x1d])
                            A(lambda e, x1=x1: e.activation(out=junk2[:], in_=x1[:], func=AF.Square, accum_out=ss2[:, 0:1]),
                              [rx1], [r_junk2, r_ss2])
                            ts(ss2[:, 1:2], ss2[:, 0:1], 1.0 / D, EPS, ALU.mult, ALU.add, [r_ss2], [r_ss2])
                            act(ss2[:, 1:2], ss2[:, 1:2], AF.Sqrt, [r_ss2], [r_ss2])
                            V(lambda e: e.reciprocal(out=ss2[:, 1:2], in_=ss2[:, 1:2]), [r_ss2], [r_ss2])
                            stt(tmpc[:], x1[:], ss2[:, 1:2], bc_s2[:], ALU.mult, ALU.mult, [rx1, r_ss2, r_bc], [r_tmpc])
                            hb_, rhb_ = hnb[si % 2], r_hnb[si % 2]
                            tt(hb_[:], tmpc[:], bc_h2[:], ALU.add, [r_tmpc, r_bc], [rhb_])
                            if DEBUG and si == 0:
                                DMA("sync", dbg["d_x1"], x1[:], [rx1], [])
                                DMA("sync", dbg["d_hn2"], hb_[:], [rhb_], [])
                            hp_, rhp_ = hnp[si % 2], r_hnp[si % 2]
                            P.add("gpsimd", lambda e, hp_=hp_, hb_=hb_: e.tensor_copy(
                                out=hp_[:].rearrange("t (kc p) -> t kc p", kc=16),
                                in_=hb_[:].rearrange("t (p kc) -> t kc p", kc=16)), [rhb_], [rhp_])
                            DMA("sync", hn2_d[t0 + s * 128:t0 + (s + 1) * 128, :], hp_[:], [rhp_], [r_hn2d])
                            for half in range(2):
                                for q in range(8):
                                    dc = half * 8 + q
                                    TR(ps_trs[half][:, q * 128:(q + 1) * 128], hb_[:, dc * 128:(dc + 1) * 128], ident_b[:],
                                       [rhb_, r_k], [r_ptr[half]])
                                act(hT16[:, half * 8:(half + 1) * 8, :], ps_trs[half][:].rearrange("p (q t) -> p q t", q=8), AF.Copy,
                                    [r_ptr[half]], [r_hT16])
                            pb, rp = genbank(POOL_C)
                            for dc in range(16):
                                MM(pb[:, 0:36], hT16[:, dc, :], wrt_b[:, dc, :], dc == 0, dc == 15, [r_hT16, r_wrt], [rp])
                            tt(lg[:], pb[:, 0:36], brt_t[:], ALU.add, [rp, r_brt], [r_lg])
                            V(lambda e: e.reduce_max(out=rt[:, 0:1], in_=lg[:, 0:4], axis=mybir.AxisListType.X), [r_lg], [r_rt])
                            ts(rt[:, 8:12], lg[:, 0:4], rt[:, 0:1], None, ALU.subtract, None, [r_lg, r_rt], [r_rt])
                            act(rt[:, 8:12], rt[:, 8:12], AF.Exp, [r_rt], [r_rt])
                            V(lambda e: e.reduce_sum(out=rt[:, 1:2], in_=rt[:, 8:12], axis=mybir.AxisListType.X), [r_rt], [r_rt])
                            V(lambda e: e.reciprocal(out=rt[:, 1:2], in_=rt[:, 1:2]), [r_rt], [r_rt])
                            ts(rt[:, 12:16], lg[:, 0:4], rt[:, 0:1], None, ALU.is_equal, None, [r_lg, r_rt], [r_rt])
                            ts(rt[:, 12:16], rt[:, 12:16], 1.0, 1e30, ALU.subtract, ALU.mult, [r_rt], [r_rt])
                            for g_ in range(4):
                                ts(rt[:, 16 + g_ * 8:24 + g_ * 8], lg[:, 4 + g_ * 8:12 + g_ * 8], rt[:, 12 + g_:13 + g_], None,
                                   ALU.add, None, [r_lg, r_rt], [r_rt])
                            EL = rt[:, 16:48]
                            V(lambda e: e.reduce_max(out=rt[:, 2:3], in_=rt[:, 16:48], axis=mybir.AxisListType.X), [r_rt], [r_rt])
                            ts(oh1s[:, si, :], EL, rt[:, 2:3], None, ALU.is_equal, None, [r_rt], [r_ohs])
                            stt(lg[:, 4:36], oh1s[:, si, :], -1e30, EL, ALU.mult, ALU.add, [r_ohs, r_rt], [r_lg])
                            V(lambda e: e.reduce_max(out=rt[:, 3:4], in_=lg[:, 4:36], axis=mybir.AxisListType.X), [r_lg], [r_rt])
                            ts(oh2s[:, si, :], lg[:, 4:36], rt[:, 3:4], None, ALU.is_equal, None, [r_lg, r_rt], [r_ohs])
                            tt(rt[:, 4:5], rt[:, 3:4], rt[:, 2:3], ALU.subtract, [r_rt], [r_rt])
                            act(rt[:, 4:5], rt[:, 4:5], AF.Exp, [r_rt], [r_rt])
                            ts(rt[:, 5:6], rt[:, 4:5], 1.0, None, ALU.add, None, [r_rt], [r_rt])
                            V(lambda e: e.reciprocal(out=rt[:, 5:6], in_=rt[:, 5:6]), [r_rt], [r_rt])
                            tt(rt[:, 6:7], rt[:, 4:5], rt[:, 5:6], ALU.mult, [r_rt], [r_rt])
                            tt(w12[:, si, 0:1], rt[:, 5:6], rt[:, 1:2], ALU.mult, [r_rt], [r_w12])
                            tt(w12[:, si, 1:2], rt[:, 6:7], rt[:, 1:2], ALU.mult, [r_rt], [r_w12])
                    P.barrier()
                with ExitStack() as esC2:
                    oh = SB(esC2, "oh", [128, NSUB, 32]); r_oh = R()
                    ohc = SB(esC2, "ohc", [128, 32]); r_ohc = R()
                    cnt = SB(esC2, "cnt", [128, 32]); r_cnt = R()
                    nbk = SB(esC2, "nbk", [128, 32]); r_nbk = R()
                    pend = SB(esC2, "pend", [128, 32]); r_pend = R()
                    base = SB(esC2, "base", [128, 32]); r_base = R()
                    tm3 = SB(esC2, "tm3", [128, NSUB, 32]); r_tm3 = R()
                    dst = SB(esC2, "dst", [128, 2 * NSUB]); r_dst = R()
                    ebk = SB(esC2, "ebk", [128, NBLK]); r_ebk = R()
                    pay = SB(esC2, "pay", [128, 2 * NSUB, 2]); r_pay = R()
                    zer = SB(esC2, "zer", [128, 128]); r_zer = R()
                    ohb = SB(esC2, "ohb", [128, NSUB, 32], BF16)
                    ohcb = SB(esC2, "ohcb", [128, 32], BF16); r_ohcb = R()
                    striub = SB(esC2, "striub", [128, 128], BF16)
                    V(lambda e: e.tensor_copy(out=striub[:], in_=striu[:]), [r_striu], [r_striu])
                    tt(oh[:], oh1s[:], oh2s[:], ALU.add, [r_ohs], [r_oh])
                    V(lambda e: e.tensor_copy(out=ohb[:], in_=oh[:]), [r_oh], [r_oh])
                    V(lambda e: e.memset(ohc[:], 0.0), [], [r_ohc])
                    V(lambda e: e.memset(ohcb[:], 0.0), [], [r_ohcb])
                    V(lambda e: e.memset(zer[:], 0.0), [], [r_zer])
                    DMA("sync", sinfo_d.rearrange("(p a) c -> p (a c)", p=128), zer[:], [r_zer], [r_sinfo])
                    for si in range(NSUB):
                        pb, rp = genbank(POOL_C)
                        MM(pb[:, 0:32], striub[:], ohb[:, si, :], True, False, [r_striu, r_oh], [rp])
                        MM(pb[:, 0:32], ones_b[:], ohcb[:], False, True, [r_k, r_ohcb], [rp])
                        act(rank[:, si, :], pb[:, 0:32], AF.Copy, [rp], [r_rank])
                        tt(ohc[:], ohc[:], oh[:, si, :], ALU.add, [r_ohc, r_oh], [r_ohc])
                        V(lambda e: e.tensor_copy(out=ohcb[:], in_=ohc[:]), [r_ohc], [r_ohcb])
                    pb, rp = genbank(POOL_C)
                    MM(pb[:, 0:32], ones_b[:], ohcb[:], True, True, [r_k, r_ohcb], [rp])
                    act(cnt[:], pb[:, 0:32], AF.Copy, [rp], [r_cnt])
                    V(lambda e: e.memset(nbk[:], 0.0), [], [r_nbk])
                    for k_ in range(16):
                        stt(nbk[:], cnt[:], float(128 * k_), nbk[:], ALU.is_gt, ALU.add, [r_cnt, r_nbk], [r_nbk])
                    V(lambda e: e.tensor_tensor_scan(out=pend[:], data0=ones_f[:, 0:32], data1=nbk[:], initial=0.0,
                                                     op0=ALU.mult, op1=ALU.add), [r_nbk, r_k], [r_pend])
                    tt(base[:], pend[:], nbk[:], ALU.subtract, [r_pend, r_nbk], [r_base])
                    ts(base[:], base[:], 128.0, None, ALU.mult, None, [r_base], [r_base])
                    for si in range(NSUB):
                        tt(tm3[:, si, :], rank[:, si, :], base[:], ALU.add, [r_rank, r_base], [r_tm3])
                    for k_, ohk in enumerate([oh1s, oh2s]):
                        tt(oh[:], tm3[:], ohk[:], ALU.mult, [r_tm3, r_ohs, r_oh], [r_oh])
                        for si in range(NSUB):
                            V(lambda e, si=si, k_=k_: e.reduce_sum(out=dst[:, 2 * si + k_:2 * si + k_ + 1], in_=oh[:, si, :],
                                                                  axis=mybir.AxisListType.X), [r_oh], [r_dst])
                    V(lambda e: e.tensor_copy(out=desti[:], in_=dst[:]), [r_dst], [r_desti])
                    V(lambda e: e.memset(ebk[:], 0.0), [], [r_ebk])
                    for e_ in range(32):
                        stt(ebk[:], cst2[:, 64:64 + NBLK], pend[:, e_:e_ + 1], ebk[:], ALU.is_ge, ALU.add, [r_cst, r_pend, r_ebk], [r_ebk])
                    ts(ebk[:], ebk[:], 31.0, 128.0, ALU.min, ALU.mult, [r_ebk], [r_ebk])
                    ts(ebk[:], ebk[:], cst2[:, 0:1], None, ALU.add, None, [r_ebk, r_cst], [r_ebk])
                    V(lambda e: e.tensor_copy(out=widx[:], in_=ebk[:]), [r_ebk], [r_widx])
                    for si in range(NSUB):
                        for k_ in range(2):
                            ts(pay[:, 2 * si + k_, 0:1], cst2[:, 0:1], float(si * 128), None, ALU.add, None, [r_cst], [r_pay])
                            V(lambda e, si=si, k_=k_: e.tensor_copy(out=pay[:, 2 * si + k_, 1:2], in_=w12[:, si, k_:k_ + 1]), [r_w12], [r_pay])
                    if DEBUG:
                        DMA("sync", dbg["d_dst"], dst[:], [r_dst], [])
                        DMA("sync", dbg["d_ebk"], ebk[:], [r_ebk], [])
                    for c_ in range(2 * NSUB):
                        sc_ = P.add("gpsimd", lambda e, c_=c_: e.indirect_dma_start(
                            out=sinfo_d[:, :], out_offset=bass.IndirectOffsetOnAxis(ap=desti[:, c_:c_ + 1], axis=0),
                            in_=pay[:, c_, :], in_offset=None), [r_pay, r_desti, r_sinfo], [r_sinfo2], dma=True)
                    P.barrier()
                with ExitStack() as esC4:
                    NW = 1
                    wst = [SB(esC4, f"wst{i}", [128, 8192]) for i in range(2)]; r_wst = [R(), R()]
                    wgs = [SB(esC4, f"wgs{i}", [128, 8192], BF16) for i in range(NW)]; r_wgs = [R() for _ in range(NW)]
                    wus = [SB(esC4, f"wus{i}", [128, 8192], BF16) for i in range(NW)]; r_wus = [R() for _ in range(NW)]
                    wds = [SB(esC4, f"wds{i}", [128, 8192], BF16) for i in range(NW)]; r_wds = [R() for _ in range(NW)]
                    sif = [SB(esC4, f"sif{i}", [128, 2]) for i in range(2)]; r_sif = [R(), R()]
                    sii = [SB(esC4, f"sii{i}", [128, 1], I32) for i in range(2)]; r_sii = [R(), R()]
                    xbr = [SB(esC4, f"xbr{i}", [128, D], BF16) for i in range(2)]; r_xbr = [R(), R()]
                    xbT = [SB(esC4, f"xbT{i}", [128, 16, 128], BF16) for i in range(2)]; r_xbT = [R(), R()]
                    sgt = SB(esC4, "sgt", [128, 512]); r_sgt = R()
                    hh = SB(esC4, "hh", [128, 512], BF16); r_hh = R()
                    hhT = SB(esC4, "hhT", [128, 4, 128], BF16); r_hhT = R()
                    ybr = [SB(esC4, f"ybr{i}", [128, D]) for i in range(2)]; r_ybr = [R(), R()]
                    for b_ in range(NBLK_RUN):
                        i2 = b_ % 2
                        DMA("sync", sif[i2][:], sinfo_d[b_ * 128:(b_ + 1) * 128, :], [r_sinfo2], [r_sif[i2]])
                        V(lambda e, i2=i2: e.tensor_copy(out=sii[i2][:], in_=sif[i2][:, 0:1]), [r_sif[i2]], [r_sii[i2]])
                        P.add("gpsimd", lambda e, i2=i2: e.indirect_dma_start(
                            out=xbr[i2][:], out_offset=None, in_=hn2_d[:, :],
                            in_offset=bass.IndirectOffsetOnAxis(ap=sii[i2][:, 0:1], axis=0)), [r_sii[i2], r_hn2d], [r_xbr[i2]], dma=True)
                        iw = b_ % NW
                        for m_, (wt, rw_, srcw, ceng) in enumerate(((wgs[iw], r_wgs[iw], weg2, "vector"), (wus[iw], r_wus[iw], weu2, "scalar"),
                                                                 (wds[iw], r_wds[iw], wed2, "vector"))):
                            kst = (3 * b_ + m_) % 2
                            P.add("gpsimd", lambda e, kst=kst, srcw=srcw, b_=b_: e.indirect_dma_start(
                                out=wst[kst][:], out_offset=None, in_=srcw[:, :],
                                in_offset=bass.IndirectOffsetOnAxis(ap=widx[:, b_:b_ + 1], axis=0)), [r_widx], [r_wst[kst]], dma=True)
                            if ceng == "vector":
                                V(lambda e, wt=wt, kst=kst: e.tensor_copy(out=wt[:], in_=wst[kst][:]), [r_wst[kst]], [rw_])
                            else:
                                act(wt[:], wst[kst][:], AF.Copy, [r_wst[kst]], [rw_])
                        for half in range(2):
                            for q in range(8):
                                kc = half * 8 + q
                                TR(ps_trs[half][:, q * 128:(q + 1) * 128], xbr[i2][:, kc * 128:(kc + 1) * 128], ident_b[:],
                                   [r_xbr[i2], r_k], [r_ptr[half]])
                            act(xbT[i2][:, half * 8:(half + 1) * 8, :], ps_trs[half][:].rearrange("p (q t) -> p q t", q=8), AF.Copy,
                                [r_ptr[half]], [r_xbT[i2]])
                        wgv = wgs[iw][:].rearrange("p (kc n) -> p kc n", kc=16)
                        wuv = wus[iw][:].rearrange("p (kc n) -> p kc n", kc=16)
                        wdv = wds[iw][:].rearrange("p (fc n) -> p fc n", fc=4)
                        pg_, rpg = genbank(POOL_C)
                        for kc in range(16):
                            MM(pg_[:], xbT[i2][:, kc, :], wgv[:, kc, :], kc == 0, kc == 15, [r_xbT[i2], r_wgs[iw]], [rpg])
                        pu_, rpu = genbank(POOL_C)
                        for kc in range(16):
                            MM(pu_[:], xbT[i2][:, kc, :], wuv[:, kc, :], kc == 0, kc == 15, [r_xbT[i2], r_wus[iw]], [rpu])
                        act(sgt[:], pg_[:], AF.Silu, [rpg], [r_sgt])
                        tt(hh[:], sgt[:], pu_[:], ALU.mult, [r_sgt, rpu], [r_hh])
                        for fc in range(4):
                            TR(ps_trs[0][:, fc * 128:(fc + 1) * 128], hh[:, fc:512:4], ident_b[:], [r_hh, r_k], [r_ptr[0]])
                        act(hhT[:], ps_trs[0][:, 0:512].rearrange("p (q t) -> p q t", q=4), AF.Copy, [r_ptr[0]], [r_hhT])
                        yb_, ryb_ = ybr[i2], r_ybr[i2]
                        for dg in range(4):
                            po, rpo = genbank(POOL_C)
                            for fc in range(4):
                                MM(po[:], hhT[:, fc, :], wdv[:, fc, dg * 512:(dg + 1) * 512], fc == 0, fc == 3, [r_hhT, r_wds[iw]], [rpo])
                            act(yb_[:, dg * 512:(dg + 1) * 512], po[:], AF.Identity, [rpo, r_sif[i2]], [ryb_], scale=sif[i2][:, 1:2])
                        DMA("sync", yb_d[b_ * 128:(b_ + 1) * 128, :], yb_[:], [ryb_], [r_ybd])
                    P.barrier()
                with ExitStack() as esC5:
                    y1 = [SB(esC5, f"y1_{i}", [128, D]) for i in range(2)]; r_y1 = [R(), R()]
                    y2 = [SB(esC5, f"y2_{i}", [128, D]) for i in range(2)]; r_y2 = [R(), R()]
                    x1r = [SB(esC5, f"x1r{i}", [128, D]) for i in range(2)]; r_x1r = [R(), R()]
                    junk3 = SB(esC5, "junk3", [128, D], BF16); r_junk3 = R()
                    ss3 = SB(esC5, "ss3", [128, 4]); r_ss3 = R()
                    for si in range(NSUB):
                        i2 = si % 2
                        P.add("gpsimd", lambda e, i2=i2, si=si: e.indirect_dma_start(
                            out=y1[i2][:], out_offset=None, in_=yb_d[:, :],
                            in_offset=bass.IndirectOffsetOnAxis(ap=desti[:, 2 * si:2 * si + 1], axis=0)), [r_desti, r_ybd], [r_y1[i2]], dma=True)
                        P.add("gpsimd", lambda e, i2=i2, si=si: e.indirect_dma_start(
                            out=y2[i2][:], out_offset=None, in_=yb_d[:, :],
                            in_offset=bass.IndirectOffsetOnAxis(ap=desti[:, 2 * si + 1:2 * si + 2], axis=0)), [r_desti, r_ybd], [r_y2[i2]], dma=True)
                        DMA("sync", x1r[i2][:], x1_d[si * 128:(si + 1) * 128, :], [r_x1d], [r_x1r[i2]])
                        tt(y1[i2][:], y1[i2][:], y2[i2][:], ALU.add, [r_y1[i2], r_y2[i2]], [r_y1[i2]])
                        tt(y1[i2][:], y1[i2][:], bc_g2[:], ALU.mult, [r_y1[i2], r_bc], [r_y1[i2]])
                        tt(x1r[i2][:], x1r[i2][:], y1[i2][:], ALU.add, [r_x1r[i2], r_y1[i2]], [r_x1r[i2]])
                        A(lambda e, i2=i2: e.activation(out=junk3[:], in_=x1r[i2][:], func=AF.Square, accum_out=ss3[:, 0:1]),
                          [r_x1r[i2]], [r_junk3, r_ss3])
                        ts(ss3[:, 1:2], ss3[:, 0:1], 1.0 / D, EPS, ALU.mult, ALU.add, [r_ss3], [r_ss3])
                        act(ss3[:, 1:2], ss3[:, 1:2], AF.Sqrt, [r_ss3], [r_ss3])
                        V(lambda e: e.reciprocal(out=ss3[:, 1:2], in_=ss3[:, 1:2]), [r_ss3], [r_ss3])
                        stt(y2[i2][:], x1r[i2][:], ss3[:, 1:2], bc_fg[:], ALU.mult, ALU.mult, [r_x1r[i2], r_ss3, r_bc, r_y2[i2]], [r_y2[i2]])
                        DMA("sync", out[si * 128:(si + 1) * 128, :], y2[i2][:], [r_y2[i2]], [])
        finally:
            HALT[0] = False
        fin = P.add("sync", None)
        fin.deps = {o.idx for o in P.ops if o.is_dma}

        with nc.Block() as block:
            run = P.finalize(sems, dsems)

            @block.sync
            def _(e):
                run("sync", e)

            @block.scalar
            def _(e):
                run("scalar", e)

            @block.vector
            def _(e):
                run("vector", e)

            @block.gpsimd
            def _(e):
                run("gpsimd", e)

            @block.tensor
            def _(e):
                run("tensor", e)
    return nc


def _col(v, n):
    return np.ascontiguousarray(np.asarray(v, np.float32).reshape(n, 128).T)


def make_in_maps(I):
    f = lambda a: np.asarray(a, np.float32)
    x, c = f(I["x"]), f(I["c"])
    w_in = f(I["w_in"])[0]
    bg = f(I["b_gates"])[0]
    conv_qk = f(I["conv_qk"])[0]
    lcw = f(I["lru_conv_w"])[0]
    consts = np.zeros((128, 256), np.float32)
    consts[:, 0:128] = np.eye(128, dtype=np.float32)
    consts[:, 128:256] = np.triu(np.ones((128, 128), np.float32))
    wout = f(I["w_out"])[0]
    perm = []
    for r in range(4):
        perm += list(range(r * 256, (r + 1) * 256)) + list(range(1024 + r * 256, 1024 + (r + 1) * 256))
    wout_p = np.ascontiguousarray(wout[perm, :])
    wrt = np.ascontiguousarray(np.concatenate([f(I["w_group"])[0], f(I["w_router"])[0]], axis=1))
    brt = np.ascontiguousarray(np.broadcast_to(np.concatenate([f(I["b_group"])[0], f(I["b_router"])[0]])[None, :], (128, 36)))
    w_ada = np.ascontiguousarray(f(I["w_ada"])[0])
    bada_c = _col(f(I["b_ada"])[0], 96)
    weg = np.ascontiguousarray(f(I["w_e_gate"])[0]).reshape(NE * 128, 8192)
    weu = np.ascontiguousarray(f(I["w_e_up"])[0]).reshape(NE * 128, 8192)
    wed = np.ascontiguousarray(f(I["w_e_down"])[0]).reshape(NE * 128, 8192)
    fing_bc = np.ascontiguousarray(np.broadcast_to(f(I["final_g"])[None, :], (128, D)))
    consts2 = np.zeros((128, 128), np.float32)
    consts2[:, 0] = np.arange(128)
    consts2[:, 64:128] = np.arange(64)[None, :]
    fing = _col(f(I["final_g"]), 16)
    maps = []
    for core in range(8):
        b, j = core // 4, core % 4
        q0, k0, v0, o0 = j * 256, 1024 + j * 256, 2048 + j * 256, 3072 + j * 256
        ii, ff = 4096 + j, 4100 + j
        xr0, gr0 = 4104 + j * 256, 4104 + 1024 + j * 256
        cols = (list(range(q0, q0 + 256)) + list(range(k0, k0 + 256)) + list(range(o0, o0 + 256)) +
                list(range(xr0, xr0 + 256)) + list(range(gr0, gr0 + 256)) + [ii] * 128 + [ff] * 128 +
                list(range(v0, v0 + 256)))
        win = np.ascontiguousarray(w_in[:, cols])
        convw = np.zeros((128, 24), np.float32)
        for cb in range(6):
            if cb < 2:
                src = conv_qk[:, j * 256 + cb * 128: j * 256 + (cb + 1) * 128]
            elif cb < 4:
                src = conv_qk[:, 1024 + j * 256 + (cb - 2) * 128: 1024 + j * 256 + (cb - 1) * 128]
            else:
                src = lcw[:, j * 256 + (cb - 4) * 128: j * 256 + (cb - 3) * 128]
            convw[:, cb * 4:(cb + 1) * 4] = src.T
        vecs = np.zeros((128, 16), np.float32)
        r0 = j * 256
        vecs[:, 0:2] = _col(f(I["lru_conv_b"])[0][r0:r0 + 256], 2)
        vecs[:, 2:4] = _col(f(I["b_lru_a"])[0][r0:r0 + 256], 2)
        vecs[:, 4:6] = _col(f(I["b_lru_x"])[0][r0:r0 + 256], 2)
        vecs[:, 6:8] = _col(f(I["lru_lambda"])[0][r0:r0 + 256], 2)
        vecs[:, 8:10] = _col(f(I["lru_norm_g"])[0][r0:r0 + 256], 2)
        vecs[:, 10:12] = _col(f(I["mh_norm_g"])[0][r0:r0 + 256], 2)
        vecs[:, 12] = bg[j]
        vecs[:, 13] = bg[4 + j]
        wl = np.zeros((128, 512), np.float32)
        for bi in range(2):
            wl[:, bi * 128:(bi + 1) * 128] = f(I["w_lru_a"])[0][2 * j + bi]
            wl[:, (2 + bi) * 128:(3 + bi) * 128] = f(I["w_lru_x"])[0][2 * j + bi]
        segmask = np.zeros((128, 4), np.float32)
        segmask[:, j] = 1.0
        maps.append({
            "x_full": np.ascontiguousarray(x[b]), "x_seg": np.ascontiguousarray(x[b, j * 2048:(j + 1) * 2048]),
            "c_col": _col(c[b], 16), "w_ada": w_ada, "bada_c": bada_c, "win": win, "convw": convw, "vecs": vecs,
            "wlru": wl, "wout": wout_p, "wrt": wrt, "brt": brt, "weg": weg, "weu": weu, "wed": wed, "fing": fing,
            "consts": consts, "segmask": segmask, "fing_bc": fing_bc, "consts2": consts2,
        })
    return maps


_NC = None
LAST = None


def kernel(**inputs):
    global _NC, LAST
    maps = make_in_maps(inputs)
    nc = build_nc()
    res = run_bass_kernel_spmd(nc, maps, core_ids=list(range(8)))
    LAST = res
    out = np.zeros((2, S, D), np.float32)
    for core in range(8):
        b, j = core // 4, core % 4
        out[b, j * 2048:(j + 1) * 2048] = np.asarray(res.results[core]["out"], np.float32)
    return out
```

```python
import os
from contextlib import ExitStack
import numpy as np
import concourse.bass as bass
import concourse.mybir as mybir
from concourse.bass_utils import run_bass_kernel_spmd

F32 = mybir.dt.float32
BF16 = mybir.dt.bfloat16
ALU = mybir.AluOpType
AF = mybir.ActivationFunctionType
ENGS = ["tensor", "vector", "scalar", "gpsimd", "sync"]

D = 2048
S = 8192
NE = 32
DE = 512
EPS = 1e-6
NT_A = int(os.environ.get("MK_NT_A", "16"))
NEXP = int(os.environ.get("MK_NEXP", "32"))
DEBUG = os.environ.get("MK_DEBUG", "0") == "1"
STOP = int(os.environ.get("MK_STOP", "0"))


HALT = [False]


def stage(k):
    if STOP == k:
        HALT[0] = True
RGROUPS = [[0, 1, 2, 3]] if os.environ.get("MK_SIM4", "0") == "1" else [[0, 1, 2, 3], [4, 5, 6, 7]]


class R:
    __slots__ = ("name", "w", "rs")

    def __init__(self, name=""):
        self.name = name
        self.w = None
        self.rs = {}


class Op:
    __slots__ = ("eng", "emit", "deps", "is_dma", "signal", "sem", "semval", "idx", "inc", "pool")


class Prog:
    def __init__(self):
        self.ops = []

    def add(self, eng, emit, reads=(), writes=(), dma=False):
        if HALT[0]:
            return Op()
        op = Op()
        op.eng, op.emit, op.is_dma = eng, emit, dma
        op.signal, op.sem, op.semval = False, None, 0
        op.idx = len(self.ops)
        op.inc, op.pool = 16, eng
        deps = set()
        for r in reads:
            if r.w is not None:
                deps.add(r.w)
        for r in writes:
            if r.w is not None:
                deps.add(r.w)
            deps.update(r.rs.values())
        deps.discard(op.idx)
        op.deps = deps
        for r in reads:
            r.rs[("dma", op.idx) if dma else eng] = op.idx
        for r in writes:
            r.w = op.idx
            r.rs = {}
        self.ops.append(op)
        return op

    def barrier(self):
        if HALT[0]:
            return
        last = {}
        dmas = set()
        for op in self.ops:
            if op.is_dma:
                dmas.add(op.idx)
            elif op.emit is not None:
                last[op.eng] = op.idx
        for e in ENGS:
            op = self.add(e, None)
            op.deps = set(last.values()) | dmas

    def finalize(self, sems, dma_sems):
        ops = self.ops
        for op in ops:
            for d in op.deps:
                p = ops[d]
                if p.is_dma or p.emit is None:
                    continue
                if p.eng == op.eng and p.eng == "tensor" and not op.is_dma and op.emit is not None:
                    continue
                p.signal = True
        cnt = {e: 0 for e in ENGS}
        dcnt = {e: [0] * len(dma_sems[e]) for e in dma_sems}
        drr = {e: 0 for e in dma_sems}
        pre_wait = {}
        for op in ops:
            if op.is_dma:
                pl = op.pool
                k = drr[pl] % len(dma_sems[pl])
                drr[pl] += 1
                if dcnt[pl][k] > 0:
                    pre_wait[op.idx] = (dma_sems[pl][k], dcnt[pl][k])
                dcnt[pl][k] += op.inc
                op.sem = dma_sems[pl][k]
                op.semval = dcnt[pl][k]
            elif op.signal:
                cnt[op.eng] += 1
                op.sem = sems[op.eng]
                op.semval = cnt[op.eng]
        streams = {e: [] for e in ENGS}
        for op in ops:
            streams[op.eng].append(op)
        self.counts = cnt

        def run_stream(engname, eng):
            waited = {}
            for op in streams[engname]:
                need = {}
                for d in op.deps:
                    p = ops[d]
                    if p.sem is None:
                        continue
                    if p.eng == engname and engname == "tensor" and not p.is_dma and not op.is_dma and op.emit is not None:
                        continue
                    key = id(p.sem)
                    if waited.get(key, 0) >= p.semval:
                        continue
                    if key not in need or need[key][1] < p.semval:
                        need[key] = (p.sem, p.semval)
                if op.idx in pre_wait:
                    s, v = pre_wait[op.idx]
                    key = id(s)
                    if waited.get(key, 0) < v and (key not in need or need[key][1] < v):
                        need[key] = (s, v)
                for key, (s, v) in need.items():
                    eng.wait_ge(s, v)
                    waited[key] = v
                if op.emit is None:
                    continue
                inst = op.emit(eng)
                if op.is_dma:
                    inst.then_inc(op.sem, op.inc)
                elif op.signal:
                    inst.then_inc(op.sem, 1)

        return run_stream


def build_nc():
    nc = bass.Bass("TRN2", target_bir_lowering=False)

    def din(name, shape, dt=F32):
        return nc.dram_tensor(name, list(shape), dt, kind="ExternalInput").ap()

    x_full = din("x_full", [S, D])
    x_seg = din("x_seg", [2048, D])
    c_col = din("c_col", [128, 16])
    w_ada = din("w_ada", [D, 6 * D])
    bada_c = din("bada_c", [128, 96])
    win = din("win", [D, 1792])
    convw = din("convw", [128, 24])
    vecs = din("vecs", [128, 16])
    wlru = din("wlru", [128, 4 * 128])
    wout = din("wout", [D, D])
    wrt = din("wrt", [D, 36])
    brt = din("brt", [128, 36])
    NOBIG = os.environ.get("MK_NOBIG", "0") == "1"
    weg2 = weu2 = wed2 = None
    if not NOBIG:
        weg2 = din("weg", [NE * 128, 8192])
        weu2 = din("weu", [NE * 128, 8192])
        wed2 = din("wed", [NE * 128, 8192])
    fing_bc = din("fing_bc", [128, D])
    consts2 = din("consts2", [128, 128])
    fing = din("fing", [128, 16])
    consts = din("consts", [128, 256])
    segmask = din("segmask", [128, 4])
    out = nc.dram_tensor("out", [2048, D], F32, kind="ExternalOutput").ap()
    cin = [nc.dram_tensor(f"cin{s}", [512, 512], BF16, kind="Internal").ap() for s in range(16)]
    cout = [nc.dram_tensor(f"cout{s}", [2048, 512], BF16, kind="Internal").ap() for s in range(16)]
    hn2_d = nc.dram_tensor("hn2_d", [2048, D], BF16, kind="Internal").ap()
    x1_d = nc.dram_tensor("x1_d", [2048, D], F32, kind="Internal").ap()
    sinfo_d = nc.dram_tensor("sinfo_d", [8192, 2], F32, kind="Internal").ap()
    yb_d = nc.dram_tensor("yb_d", [8192, D], F32, kind="Internal").ap()
    r_hn2d, r_x1d, r_sinfo, r_sinfo2, r_ybd = R(), R(), R(), R(), R()
    NBLK_RUN = int(os.environ.get("MK_NBLK", "64"))
    dbg = {}
    if DEBUG:
        for nm, shp, dt in [("d_mod", [128, 96], F32), ("d_hnT", [128, 16 * 512], BF16), ("d_y", [128, 4 * 512], BF16),
                            ("d_x1T", [128, 16 * 512], F32), ("d_hn2T", [128, 16 * 512], BF16), ("d_cw", [128, 4 * 32], F32),
                            ("d_q", [128, 2 * 512], BF16), ("d_k", [128, 2 * 512], BF16), ("d_hm", [128, 2 * 512], F32),
                            ("d_x2T", [128, 16 * 512], F32), ("d_x1", [128, D], F32), ("d_hn2", [128, D], BF16),
                            ("d_dst", [128, 32], F32), ("d_ebk", [128, 64], F32)]:
            dbg[nm] = nc.dram_tensor(nm, shp, dt, kind="ExternalOutput").ap()

    P = Prog()
    V = lambda f, rd, wr: P.add("vector", f, rd, wr)
    A = lambda f, rd, wr: P.add("scalar", f, rd, wr)
    G = lambda f, rd, wr: P.add("gpsimd", f, rd, wr)

    def MM(o, l, r, st, sp, rd, wr):
        return P.add("tensor", lambda e: e.matmul(o, l, r, start=st, stop=sp), rd, wr)

    def TR(o, i, ident, rd, wr):
        return P.add("tensor", lambda e: e.transpose(o, i, ident), rd, wr)

    def DMA(eng, o, i, rd, wr):
        return P.add(eng, lambda e: e.dma_start(out=o, in_=i), rd, wr, dma=True)

    def act(o, i, f, rd, wr, **kw):
        return A(lambda e: e.activation(out=o, in_=i, func=f, **kw), rd, wr)

    def tt(o, a, b, op, rd, wr, eng="vector"):
        return P.add(eng, lambda e: e.tensor_tensor(out=o, in0=a, in1=b, op=op), rd, wr)

    def ts(o, a, s1, s2, op0, op1, rd, wr, eng="vector"):
        if op1 is None:
            return P.add(eng, lambda e: e.tensor_scalar(out=o, in0=a, scalar1=s1, scalar2=None, op0=op0), rd, wr)
        return P.add(eng, lambda e: e.tensor_scalar(out=o, in0=a, scalar1=s1, scalar2=s2, op0=op0, op1=op1), rd, wr)

    def stt(o, a, s, b, op0, op1, rd, wr):
        return V(lambda e: e.scalar_tensor_tensor(out=o, in0=a, scalar=s, in1=b, op0=op0, op1=op1), rd, wr)

    with ExitStack() as es0:
        sems = {e: es0.enter_context(nc.semaphore("s_" + e)) for e in ENGS}
        dsems = {e: [es0.enter_context(nc.semaphore(f"d_{e}{i}")) for i in range(8)] for e in ["sync", "gpsimd"]}
        dsems["cc"] = [es0.enter_context(nc.semaphore("cc_sem"))]

        def SB(es, name, shape, dt=F32):
            return es.enter_context(nc.sbuf_tensor(name, list(shape), dt))

        cst = SB(es0, "cst", [128, 256]); r_cst = R()
        cst2 = SB(es0, "cst2", [128, 128])
        ident_f = cst[:, 0:128]
        tri = cst[:, 128:256]
        ident_b = SB(es0, "ident_b", [128, 128], BF16)
        ones_f = SB(es0, "ones_f", [128, 128])
        ones_b = SB(es0, "ones_b", [128, 128], BF16)
        c256_b = SB(es0, "c256_b", [128, 128], BF16)
        c128_b = SB(es0, "c128_b", [128, 128], BF16)
        c2048_b = SB(es0, "c2048_b", [128, 128], BF16)
        modc = SB(es0, "modc", [128, 96]); r_modc = R()
        modp = SB(es0, "modp", [128, 32]); r_modp = R()
        vec = SB(es0, "vec", [128, 16]); r_vec = R()
        lsv = SB(es0, "lsv", [128, 8]); r_lsv = R()
        cwv = SB(es0, "cwv", [128, 24]); r_cwv = R()
        fg = SB(es0, "fg", [128, 16]); r_fg = R()
        smk = SB(es0, "smk", [128, 4]); r_smk = R()
        brt_t = SB(es0, "brt_t", [128, 36]); r_brt = R()
        wrt_b = SB(es0, "wrt_b", [128, 16, 36], BF16); r_wrt = R()
        ps_trs = [es0.enter_context(nc.psum_tensor(f"ps_tr{i}", [128, 1024], BF16)) for i in range(2)]
        r_ptr = [R(), R()]
        psb = [es0.enter_context(nc.psum_tensor(f"psb{i}", [128, 512], F32)) for i in range(6)]
        r_psb = [R() for _ in range(6)]
        gen_rr = [0]

        def genbank(pool):
            i = pool[gen_rr[0] % len(pool)]
            gen_rr[0] += 1
            return psb[i], r_psb[i]

        DMA("sync", cst[:], consts, [], [r_cst])
        DMA("sync", cst2[:], consts2, [], [r_cst])
        DMA("sync", vec[:], vecs, [], [r_vec])
        DMA("sync", cwv[:], convw, [], [r_cwv])
        DMA("sync", fg[:], fing, [], [r_fg])
        DMA("sync", smk[:], segmask, [], [r_smk])
        DMA("sync", brt_t[:], brt, [], [r_brt])
        DMA("sync", modc[:], bada_c, [], [r_modc])
        DMA("gpsimd", wrt_b[:], wrt.rearrange("(kc p) n -> p kc n", p=128), [], [r_wrt])
        r_k = R()
        V(lambda e: e.memset(ones_f[:], 1.0), [], [r_k])
        V(lambda e: e.memset(ones_b[:], 1.0), [], [r_k])
        V(lambda e: e.memset(c256_b[:], 1.0 / 256), [], [r_k])
        V(lambda e: e.memset(c128_b[:], 1.0 / 128), [], [r_k])
        V(lambda e: e.memset(c2048_b[:], 1.0 / 2048), [], [r_k])
        V(lambda e: e.tensor_copy(out=ident_b[:], in_=ident_f), [r_cst], [r_k])

        try:
            with ExitStack() as esA:
                POOL_A = [0, 1, 2]
                B_S, B_N, B_C0 = 3, 4, 5
                win_b = SB(esA, "win_b", [128, 16, 1792], BF16); r_win = R()
                for kc in range(16):
                    DMA("gpsimd", win_b[:, kc, :], win[kc * 128:(kc + 1) * 128, :], [], [r_win])
                esL = esA.enter_context(ExitStack())
                ccol = SB(esL, "ccol", [128, 16]); r_cc = R()
                ccol_b = SB(esL, "ccol_b", [128, 16], BF16)
                DMA("sync", ccol[:], c_col, [], [r_cc])
                act(ccol_b[:], ccol[:], AF.Silu, [r_cc], [r_cc])
                NGW = 256
                wa = [SB(esL, f"wa{i}", [128, 16, NGW], BF16) for i in range(2)]
                r_wa = [R(), R()]
                rowb = SB(esL, "rowb", [1, NGW]); r_row = R()
                for ng in range(6 * D // NGW):
                    bi = ng % 2
                    for h in range(2):
                        DMA("gpsimd", wa[bi][:, h * 8:(h + 1) * 8, :],
                            w_ada[h * 1024:(h + 1) * 1024, ng * NGW:(ng + 1) * NGW].rearrange("(kc p) n -> p kc n", p=128),
                            [], [r_wa[bi]])
                    pb, rp = genbank(POOL_A)
                    for kc in range(16):
                        MM(pb[0:1, 0:NGW], ccol_b[:, kc:kc + 1], wa[bi][:, kc, :], kc == 0, kc == 15, [r_cc, r_wa[bi]], [rp])
                    act(rowb[:], pb[0:1, 0:NGW], AF.Copy, [rp], [r_row])
                    pb2, rp2 = genbank(POOL_A)
                    nq = NGW // 128
                    for q in range(nq):
                        MM(pb2[:, q:q + 1], rowb[0:1, q * 128:(q + 1) * 128], ones_f[0:1, 0:1], True, True, [r_row, r_k], [rp2])
                    c0 = ng * nq
                    tt(modc[:, c0:c0 + nq], modc[:, c0:c0 + nq], pb2[:, 0:nq], ALU.add, [rp2, r_modc], [r_modc])
                ts(modp[:, 0:16], modc[:, 16:32], 1.0, None, ALU.add, None, [r_modc], [r_modp])
                ts(modp[:, 16:32], modc[:, 64:80], 1.0, None, ALU.add, None, [r_modc], [r_modp])
                if DEBUG:
                    DMA("sync", dbg["d_mod"], modc[:], [r_modc], [])
                P.barrier()
                esL.close()
                stage(1)
                lt = SB(esA, "lt", [128, 2])
                act(lt[:], vec[:, 6:8], AF.Exp, [r_vec], [r_lsv], scale=-1.0)
                ts(lt[:], lt[:], 1.0, None, ALU.add, None, [r_lsv], [r_lsv])
                act(lt[:], lt[:], AF.Ln, [r_lsv], [r_lsv])
                ts(lsv[:, 0:2], lt[:], -8.0, None, ALU.mult, None, [r_lsv], [r_lsv])
                ts(lsv[:, 2:4], lt[:], -16.0, None, ALU.mult, None, [r_lsv], [r_lsv])
                wl_f = SB(esA, "wl_f", [128, 512]); r_wl = R()
                wl_b = SB(esA, "wl_b", [128, 512], BF16)
                DMA("sync", wl_f[:], wlru, [], [r_wl])
                V(lambda e: e.tensor_copy(out=wl_b[:], in_=wl_f[:]), [r_wl], [r_wl])

                xt = [SB(esA, f"xt{i}", [128, D]) for i in range(2)]; r_xt = [R() for _ in range(2)]
                junk = SB(esA, "junk", [128, D], BF16); r_junk = R()
                xs = SB(esA, "xs", [128, 4, D], BF16); r_xs = [R() for _ in range(4)]
                ssq = SB(esA, "ssq", [128, 8]); r_ssq = R()
                hnT = SB(esA, "hnT", [128, 16, 512], BF16); r_hnT = R()
                raw = SB(esA, "raw", [128, 515]); r_raw = R()
                halo = SB(esA, "halo", [128, 6, 3]); r_halo = [R() for _ in range(6)]
                acc = SB(esA, "acc", [128, 512]); r_acc = R()
                tmpA = SB(esA, "tmpA", [128, 512]); r_tmpA = R()
                qT = SB(esA, "qT", [128, 2, 512], BF16); r_q = R()
                kT = SB(esA, "kT", [128, 2, 512], BF16); r_kk = R()
                q2T = SB(esA, "q2T", [128, 2, 512], BF16); r_q2 = R()
                og = SB(esA, "og", [128, 2, 512]); r_og = R()
                xrf = SB(esA, "xrf", [128, 2, 512]); r_xrf = R()
                xrb = SB(esA, "xrb", [128, 2, 512], BF16)
                gl = SB(esA, "gl", [128, 2, 512]); r_gl = R()
                ibc = SB(esA, "ibc", [128, 512]); r_ibc = R()
                nlf = SB(esA, "nlf", [128, 512]); r_nlf = R()
                nbb = SB(esA, "nbb", [128, 512]); r_nbb = R()
                ebb = SB(esA, "ebb", [128, 512]); r_ebb = R()
                gtk = SB(esA, "gtk", [128, 32]); r_gtk = R()
                zt = SB(esA, "zt", [128, 512]); r_zt = R()
                dm = SB(esA, "dm", [128, 512]); r_dm = R()
                tri4 = SB(esA, "tri4", [128, 512]); r_tri4 = R()
                PT = SB(esA, "PT", [128, 512], BF16); r_PT = R()
                vext = SB(esA, "vext", [128, 4, 258], BF16); r_vext = [R() for _ in range(4)]
                kw = SB(esA, "kw", [128, 4, 256], BF16); r_kw = [R() for _ in range(4)]
                Cf = SB(esA, "Cf", [128, 2, 257]); r_Cf = R()
                Cb = SB(esA, "Cb", [128, 2, 256], BF16); r_Cb = R()
                nbc = SB(esA, "nbc", [128, 2, 128], BF16); r_nbc = R()
                dab = SB(esA, "dab", [128, 128]); r_dab = R()
                hmT = SB(esA, "hmT", [128, 2, 512]); r_hm = R()
                sqb = SB(esA, "sqb", [128, 2, 512], BF16); r_sq = R()
                rsb = SB(esA, "rsb", [128, 512]); r_rs = R()
                lr = SB(esA, "lr", [128, 512]); r_lr = R()
                li = SB(esA, "li", [128, 512]); r_li = R()
                la, r_la = zt, r_zt
                lm, r_lm = dm, r_dm
                lh = SB(esA, "lh", [128, 512]); r_lh = R()
                hprev = SB(esA, "hprev", [128, 2]); r_hp = R()
                ytile = [SB(esA, f"ytile{i}", [128, 4, 512], BF16) for i in range(2)]; r_yt = [R(), R()]
                r_cin = [R() for _ in range(16)]
                r_cout = [R() for _ in range(16)]

                V(lambda e: e.memset(halo[:], 0.0), [], r_halo)
                V(lambda e: e.memset(Cf[:], 0.0), [], [r_Cf])
                V(lambda e: e.memset(Cb[:], 0.0), [], [r_Cb])
                V(lambda e: e.memset(nbc[:], 0.0), [], [r_nbc])
                V(lambda e: e.memset(hprev[:], 0.0), [], [r_hp])
                V(lambda e: e.memset(vext[:], 1.0), [], r_vext)
                for c in range(4):
                    V(lambda e, c=c: e.tensor_copy(out=tri4[:, c * 128:(c + 1) * 128], in_=tri), [r_cst], [r_tri4])

                xrr = [0]
                for it in range(NT_A):
                    for s in range(4):
                        xb = xrr[0] % 2
                        xrr[0] += 1
                        row0 = (it * 4 + s) * 128
                        DMA("sync", xt[xb][:], x_full[row0:row0 + 128, :], [], [r_xt[xb]])
                        A(lambda e, xb=xb, s=s: e.activation(out=junk[:], in_=xt[xb][:], func=AF.Square, accum_out=ssq[:, s:s + 1]),
                          [r_xt[xb]], [r_junk, r_ssq])
                        ts(ssq[:, 4 + s:5 + s], ssq[:, s:s + 1], 1.0 / D, EPS, ALU.mult, ALU.add, [r_ssq], [r_ssq])
                        act(ssq[:, 4 + s:5 + s], ssq[:, 4 + s:5 + s], AF.Sqrt, [r_ssq], [r_ssq])
                        V(lambda e, s=s: e.reciprocal(out=ssq[:, 4 + s:5 + s], in_=ssq[:, 4 + s:5 + s]), [r_ssq], [r_ssq])
                        ts(xs[:, s, :], xt[xb][:], ssq[:, 4 + s:5 + s], None, ALU.mult, None, [r_ssq, r_xt[xb]], [r_xs[s]])
                    for dc in range(16):
                        h = dc % 2
                        for s in range(4):
                            TR(ps_trs[h][:, s * 128:(s + 1) * 128], xs[:, s, dc * 128:(dc + 1) * 128], ident_b[:],
                               [r_xs[s], r_k], [r_ptr[h]])
                        act(hnT[:, dc, :], ps_trs[h][:, 0:512], AF.Identity, [r_ptr[h], r_modp, r_modc], [r_hnT],
                            scale=modp[:, dc:dc + 1], bias=modc[:, dc:dc + 1])
                    stage(2)
                    if DEBUG and it == 0:
                        DMA("sync", dbg["d_hnT"], hnT[:].rearrange("p a b -> p (a b)"), [r_hnT], [])
                    yt = ytile[it % 2]
                    r_y = r_yt[it % 2]
                    for blk in range(12):
                        pb, rp = genbank(POOL_A)
                        for kc in range(16):
                            MM(pb[:], win_b[:, kc, blk * 128:(blk + 1) * 128], hnT[:, kc, :], kc == 0, kc == 15, [r_win, r_hnT], [rp])
                        if blk < 4 or 6 <= blk < 8:
                            cb = blk if blk < 4 else blk - 2
                            V(lambda e, cb=cb: e.tensor_copy(out=raw[:, 0:3], in_=halo[:, cb, :]), [r_halo[cb]], [r_raw])
                            act(raw[:, 3:515], pb[:], AF.Copy, [rp], [r_raw])
                            ts(acc[:], raw[:, 3:515], cwv[:, cb * 4 + 3:cb * 4 + 4], None, ALU.mult, None, [r_raw, r_cwv], [r_acc])
                            for j in range(3):
                                stt(acc[:], raw[:, j:j + 512], cwv[:, cb * 4 + j:cb * 4 + j + 1], acc[:], ALU.mult, ALU.add,
                                    [r_raw, r_acc], [r_acc])
                            V(lambda e, cb=cb: e.tensor_copy(out=halo[:, cb, :], in_=raw[:, 512:515]), [r_raw], [r_halo[cb]])
                            if cb < 2:
                                act(qT[:, cb, :], acc[:], AF.Silu, [r_acc], [r_q])
                            elif cb < 4:
                                act(tmpA[:], acc[:], AF.Sigmoid, [r_acc], [r_tmpA])
                                stt(kT[:, cb - 2, :], tmpA[:], 1.0 / 16, acc[:], ALU.mult, ALU.mult, [r_tmpA, r_acc], [r_kk])
                            else:
                                bi = cb - 4
                                act(xrf[:, bi, :], acc[:], AF.Identity, [r_acc, r_vec], [r_xrf], bias=vec[:, 0 + bi:1 + bi])
                                V(lambda e, bi=bi: e.tensor_copy(out=xrb[:, bi, :], in_=xrf[:, bi, :]), [r_xrf], [r_xrf])
                        elif blk < 6:
                            act(og[:, blk - 4, :], pb[:], AF.Sigmoid, [rp], [r_og])
                        elif blk < 10:
                            bi = blk - 8
                            act(gl[:, bi, :], pb[:], AF.Gelu_apprx_tanh, [rp], [r_gl])
                        elif blk == 10:
                            act(ibc[:], pb[:], AF.Identity, [rp, r_vec], [r_ibc], bias=vec[:, 12:13])
                        else:
                            act(nlf[:], pb[:], AF.Identity, [rp, r_vec], [r_nlf], bias=vec[:, 13:14])
                            act(nlf[:], nlf[:], AF.Exp, [r_nlf], [r_nlf], scale=-1.0)
                            ts(nlf[:], nlf[:], 1.0, None, ALU.add, None, [r_nlf], [r_nlf])
                            act(nlf[:], nlf[:], AF.Ln, [r_nlf], [r_nlf])
                    if DEBUG and it == 0:
                        DMA("sync", dbg["d_q"], qT[:].rearrange("p a b -> p (a b)"), [r_q], [])
                        DMA("sync", dbg["d_k"], kT[:].rearrange("p a b -> p (a b)"), [r_kk], [])
                    stage(3)
                    for s in range(4):
                        pb, rp = genbank(POOL_A)
                        for kc in range(16):
                            MM(pb[:, 0:256], hnT[:, kc, s * 128:(s + 1) * 128], win_b[:, kc, 1536:1792], kc == 0, kc == 15,
                               [r_win, r_hnT], [rp])
                        act(vext[:, s, 0:256], pb[:, 0:256], AF.Copy, [rp], [r_vext[s]])
                    for c in range(4):
                        V(lambda e, c=c: e.tensor_tensor_scan(out=nbb[:, c * 128:(c + 1) * 128], data0=ones_f[:],
                                                              data1=nlf[:, c * 128:(c + 1) * 128], initial=0.0,
                                                              op0=ALU.mult, op1=ALU.add), [r_nlf, r_k], [r_nbb])
                    for c in range(4):
                        cs = slice(c * 128, (c + 1) * 128)
                        tt(zt[:, cs], ibc[:, cs], ident_f, ALU.mult, [r_ibc, r_cst], [r_zt])
                        V(lambda e, c=c, cs=cs: e.reduce_sum(out=gtk[:, c:c + 1], in_=zt[:, cs], axis=mybir.AxisListType.X),
                          [r_zt], [r_gtk])
                        tt(dm[:, cs], nbb[:, cs], ident_f, ALU.mult, [r_nbb, r_cst], [r_dm])
                        V(lambda e, c=c, cs=cs: e.reduce_sum(out=gtk[:, 8 + c:9 + c], in_=dm[:, cs], axis=mybir.AxisListType.X),
                          [r_dm], [r_gtk])
                        V(lambda e, c=c: e.tensor_copy(out=gtk[:, 12 + c:13 + c], in_=nbb[:, c * 128 + 127:c * 128 + 128]),
                          [r_nbb], [r_gtk])
                    act(ebb[:], nbb[:], AF.Exp, [r_nbb], [r_ebb], scale=-1.0)
                    for dk in range(2):
                        tt(q2T[:, dk, :], qT[:, dk, :], ebb[:], ALU.mult, [r_q, r_ebb], [r_q2])
                    tt(gtk[:, 24:28], gtk[:, 12:16], gtk[:, 8:12], ALU.subtract, [r_gtk], [r_gtk])
                    tt(gtk[:, 24:28], gtk[:, 0:4], gtk[:, 24:28], ALU.subtract, [r_gtk], [r_gtk])
                    act(gtk[:, 16:20], gtk[:, 24:28], AF.Exp, [r_gtk], [r_gtk])
                    act(gtk[:, 20:24], gtk[:, 12:16], AF.Exp, [r_gtk], [r_gtk], scale=-1.0)
                    stage(4)
                    for c in range(4):
                        cs = slice(c * 128, (c + 1) * 128)
                        for dk in range(2):
                            MM(psb[B_S][:, cs], kT[:, dk, cs], qT[:, dk, cs], dk == 0, dk == 1, [r_kk, r_q], [r_psb[B_S]])
                        ts(zt[:, cs], nbb[:, cs], gtk[:, 8 + c:9 + c], 0.0, ALU.subtract, ALU.max, [r_nbb, r_gtk], [r_zt])
                        act(dm[:, cs], zt[:, cs], AF.Exp, [r_zt, r_gtk], [r_dm], scale=-1.0, bias=gtk[:, c:c + 1])
                    tt(dm[:], dm[:], tri4[:], ALU.mult, [r_dm, r_tri4], [r_dm])
                    tt(PT[:], psb[B_S][:], dm[:], ALU.mult, [r_psb[B_S], r_dm], [r_PT])
                    for c in range(4):
                        cs = slice(c * 128, (c + 1) * 128)
                        h = c % 2
                        for dk in range(2):
                            TR(ps_trs[h][:, dk * 128:(dk + 1) * 128], kT[:, dk, cs], ident_b[:], [r_kk, r_k], [r_ptr[h]])
                        act(kw[:, c, :], ps_trs[h][:, 0:256], AF.Identity, [r_ptr[h], r_gtk], [r_kw[c]], scale=gtk[:, 16 + c:17 + c])
                    stage(5)
                    for c in range(4):
                        cs = slice(c * 128, (c + 1) * 128)
                        pN, rN = psb[B_N], r_psb[B_N]
                        for e_ in range(2):
                            es_ = slice(e_ * 128, (e_ + 1) * 128)
                            MM(pN[:, es_], vext[:, c, es_], PT[:, cs], True, False, [r_vext[c], r_PT], [rN])
                            MM(pN[:, es_], Cb[:, 0, es_], q2T[:, 0, cs], False, False, [r_Cb, r_q2], [rN])
                            MM(pN[:, es_], Cb[:, 1, es_], q2T[:, 1, cs], False, True, [r_Cb, r_q2], [rN])
                        MM(pN[:, 256:384], ones_b[:], PT[:, cs], True, False, [r_k, r_PT], [rN])
                        MM(pN[:, 256:384], nbc[:, 0, :], q2T[:, 0, cs], False, False, [r_nbc, r_q2], [rN])
                        MM(pN[:, 256:384], nbc[:, 1, :], q2T[:, 1, cs], False, True, [r_nbc, r_q2], [rN])
                        act(dab[:], pN[:, 256:384], AF.Abs, [rN], [r_dab])
                        ts(dab[:], dab[:], 1.0, None, ALU.max, None, [r_dab], [r_dab])
                        V(lambda e: e.reciprocal(out=dab[:], in_=dab[:]), [r_dab], [r_dab])
                        for e_ in range(2):
                            tt(hmT[:, e_, cs], pN[:, e_ * 128:(e_ + 1) * 128], dab[:], ALU.mult, [rN, r_dab], [r_hm])
                        for dk in range(2):
                            bk = B_C0
                            MM(psb[bk][:, 0:257], kw[:, c, dk * 128:(dk + 1) * 128], vext[:, c, 0:257], True, True,
                               [r_kw[c], r_vext[c]], [r_psb[bk]])
                            stt(Cf[:, dk, :], Cf[:, dk, :], gtk[:, 20 + c:21 + c], psb[bk][:, 0:257], ALU.mult, ALU.add,
                                [r_Cf, r_gtk, r_psb[bk]], [r_Cf])
                            act(Cb[:, dk, :], Cf[:, dk, 0:256], AF.Copy, [r_Cf], [r_Cb])
                            ts(nbc[:, dk, :], ones_f[:], Cf[:, dk, 256:257], None, ALU.mult, None, [r_Cf, r_k], [r_nbc])
                    if DEBUG and it == 0:
                        DMA("sync", dbg["d_hm"], hmT[:].rearrange("p a b -> p (a b)"), [r_hm], [])
                    stage(6)
                    act(sqb[:], hmT[:], AF.Square, [r_hm], [r_sq])
                    pb, rp = genbank(POOL_A)
                    MM(pb[:], c256_b[:], sqb[:, 0, :], True, False, [r_k, r_sq], [rp])
                    MM(pb[:], c256_b[:], sqb[:, 1, :], False, True, [r_k, r_sq], [rp])
                    ts(rsb[:], pb[:], EPS, None, ALU.add, None, [rp], [r_rs])
                    act(rsb[:], rsb[:], AF.Sqrt, [r_rs], [r_rs])
                    V(lambda e: e.reciprocal(out=rsb[:], in_=rsb[:]), [r_rs], [r_rs])
                    for e_ in range(2):
                        stt(tmpA[:], hmT[:, e_, :], vec[:, 10 + e_:11 + e_], rsb[:], ALU.mult, ALU.mult, [r_hm, r_vec, r_rs], [r_tmpA])
                        tt(yt[:, e_, :], tmpA[:], og[:, e_, :], ALU.mult, [r_tmpA, r_og], [r_y])
                    stage(7)
                    for bi in range(2):
                        pa, rpa = genbank(POOL_A)
                        MM(pa[:], wl_b[:, bi * 128:(bi + 1) * 128], xrb[:, bi, :], True, True, [r_wl, r_xrf], [rpa])
                        px, rpx = genbank(POOL_A)
                        MM(px[:], wl_b[:, (2 + bi) * 128:(3 + bi) * 128], xrb[:, bi, :], True, True, [r_wl, r_xrf], [rpx])
                        act(lr[:], pa[:], AF.Sigmoid, [rpa, r_vec], [r_lr], bias=vec[:, 2 + bi:3 + bi])
                        act(li[:], px[:], AF.Sigmoid, [rpx, r_vec], [r_li], bias=vec[:, 4 + bi:5 + bi])
                        act(la[:], lr[:], AF.Exp, [r_lr, r_lsv], [r_la], scale=lsv[:, bi:bi + 1])
                        act(lm[:], lr[:], AF.Exp, [r_lr, r_lsv], [r_lm], scale=lsv[:, 2 + bi:3 + bi])
                        ts(lm[:], lm[:], -1.0, 1.0, ALU.mult, ALU.add, [r_lm], [r_lm])
                        act(lm[:], lm[:], AF.Sqrt, [r_lm], [r_lm])
                        tt(lm[:], lm[:], li[:], ALU.mult, [r_lm, r_li], [r_lm])
                        tt(lm[:], lm[:], xrf[:, bi, :], ALU.mult, [r_lm, r_xrf], [r_lm])
                        V(lambda e, bi=bi: e.tensor_tensor_scan(out=lh[:], data0=la[:], data1=lm[:], initial=hprev[:, bi:bi + 1],
                                                                op0=ALU.mult, op1=ALU.add), [r_la, r_lm, r_hp], [r_lh])
                        V(lambda e, bi=bi: e.tensor_copy(out=hprev[:, bi:bi + 1], in_=lh[:, 511:512]), [r_lh], [r_hp])
                        tt(lh[:], lh[:], gl[:, bi, :], ALU.mult, [r_lh, r_gl], [r_lh])
                        act(sqb[:, 0, :], lh[:], AF.Square, [r_lh], [r_sq])
                        pb, rp = genbank(POOL_A)
                        MM(pb[:], c128_b[:], sqb[:, 0, :], True, True, [r_k, r_sq], [rp])
                        ts(rsb[:], pb[:], EPS, None, ALU.add, None, [rp], [r_rs])
                        act(rsb[:], rsb[:], AF.Sqrt, [r_rs], [r_rs])
                        V(lambda e: e.reciprocal(out=rsb[:], in_=rsb[:]), [r_rs], [r_rs])
                        stt(yt[:, 2 + bi, :], lh[:], vec[:, 8 + bi:9 + bi], rsb[:], ALU.mult, ALU.mult, [r_lh, r_vec, r_rs], [r_y])
                    stage(8)
                    DMA("sync", cin[it].rearrange("(blk p) t -> p blk t", p=128), yt[:], [r_y], [r_cin[it]])
                    if DEBUG and it == 0:
                        DMA("sync", dbg["d_y"], yt[:].rearrange("p a b -> p (a b)"), [r_y], [])
                    cc = P.add("gpsimd", lambda e, it=it: e.collective_compute(
                        "AllGather", ALU.bypass, replica_groups=RGROUPS,
                        ins=[cin[it].opt()], outs=[cout[it].opt()]), [r_cin[it]], [r_cout[it]], dma=True)
                    cc.inc, cc.pool = 1, "cc"
                    stage(9)
                P.barrier()

            with ExitStack() as esC:
                POOL_C = [0, 1, 2, 3, 4, 5]
                I32 = mybir.dt.int32
                NSUB = 16
                NBLK = 64
                bc_g1 = SB(esC, "bc_g1", [128, D]); bc_s2 = SB(esC, "bc_s2", [128, D]); bc_h2 = SB(esC, "bc_h2", [128, D])
                bc_g2 = SB(esC, "bc_g2", [128, D]); bc_fg = SB(esC, "bc_fg", [128, D]); r_bc = R()
                oh1s = SB(esC, "oh1s", [128, NSUB, 32]); oh2s = SB(esC, "oh2s", [128, NSUB, 32]); r_ohs = R()
                w12 = SB(esC, "w12", [128, NSUB, 2]); r_w12 = R()
                rank = SB(esC, "rank", [128, NSUB, 32]); r_rank = R()
                desti = SB(esC, "desti", [128, 2 * NSUB], I32); r_desti = R()
                widx = SB(esC, "widx", [128, NBLK], I32); r_widx = R()
                striu = SB(esC, "striu", [128, 128]); r_striu = R()
                diag = [SB(esC, f"diag{i}", [128, 128]) for i in range(2)]; r_diag = [R(), R()]
                tt(striu[:], tri, ident_f, ALU.subtract, [r_cst], [r_striu])
                DMA("sync", bc_fg[:], fing_bc, [], [r_bc])

                dgh = [SB(esC, f"dgh{i}", [128, 128], BF16) for i in range(2)]
                dgl = [SB(esC, f"dgl{i}", [128, 128], BF16) for i in range(2)]

                def bcast(dst, col0):
                    for dg in range(4):
                        pb, rp = genbank(POOL_C)
                        for q in range(4):
                            ci = dg * 4 + q
                            dgt, rdg = diag[ci % 2], r_diag[ci % 2]
                            hi_, lo_ = dgh[ci % 2], dgl[ci % 2]
                            src = modp if col0 < 0 else modc
                            c_ = (16 + ci) if col0 < 0 else (col0 + ci)
                            ts(dgt[:], ident_f, src[:, c_:c_ + 1], None, ALU.mult, None, [r_cst, r_modc, r_modp], [rdg])
                            V(lambda e, hi_=hi_, dgt=dgt: e.tensor_copy(out=hi_[:], in_=dgt[:]), [rdg], [rdg])
                            tt(dgt[:], dgt[:], hi_[:], ALU.subtract, [rdg], [rdg])
                            V(lambda e, lo_=lo_, dgt=dgt: e.tensor_copy(out=lo_[:], in_=dgt[:]), [rdg], [rdg])
                            MM(pb[:, q * 128:(q + 1) * 128], ones_b[:], hi_[:], True, False, [r_k, rdg], [rp])
                            MM(pb[:, q * 128:(q + 1) * 128], ones_b[:], lo_[:], False, True, [r_k, rdg], [rp])
                        act(dst[:, dg * 512:(dg + 1) * 512], pb[:], AF.Copy, [rp], [r_bc])
                bcast(bc_g1, 32)
                bcast(bc_s2, -1)
                bcast(bc_h2, 48)
                bcast(bc_g2, 80)

                with ExitStack() as esC1:
                    xl = [SB(esC1, f"xl{i}", [128, D]) for i in range(2)]; r_xl = [R(), R()]
                    ytl = SB(esC1, "ytl", [128, 16, 512], BF16); r_ytl = R()
                    ytmp = SB(esC1, "ytmp", [128, 16, 512], BF16); r_ytmp = R()
                    wo = [SB(esC1, f"wo{i}", [128, 16, 512], BF16) for i in range(4)]; r_wo = R()
                    junk2 = SB(esC1, "junk2", [128, D], BF16); r_junk2 = R()
                    tmpc = SB(esC1, "tmpc", [128, D]); r_tmpc = R()
                    hnb = [SB(esC1, f"hnb{i}", [128, D], BF16) for i in range(2)]; r_hnb = [R(), R()]
                    hnp = [SB(esC1, f"hnp{i}", [128, D], BF16) for i in range(2)]; r_hnp = [R(), R()]
                    hT16 = SB(esC1, "hT16", [128, 16, 128], BF16); r_hT16 = R()
                    ss2 = SB(esC1, "ss2", [128, 4]); r_ss2 = R()
                    lg = SB(esC1, "lg", [128, 36]); r_lg = R()
                    rt = SB(esC1, "rt", [128, 64]); r_rt = R()
                    for dgp in range(4):
                        for h in range(2):
                            DMA("gpsimd", wo[dgp][:, h * 8:(h + 1) * 8, :],
                                wout[h * 1024:(h + 1) * 1024, dgp * 512:(dgp + 1) * 512].rearrange("(kc p) n -> p kc n", p=128),
                                [], [r_wo])
                    for tt_ in range(4):
                        t0 = tt_ * 512
                        for sgm in range(min(4, NT_A // 4)):
                            src = cout[4 * sgm + tt_].rearrange("(kc p) t -> p kc t", p=128)
                            r_src = r_cout[4 * sgm + tt_]
                            DMA("sync", ytmp[:], src, [r_src], [r_ytmp])
                            if sgm == 0:
                                ts(ytl[:], ytmp[:], smk[:, 0:1], None, ALU.mult, None, [r_ytmp, r_smk], [r_ytl])
                            else:
                                stt(ytl[:].rearrange("p a b -> p (a b)"), ytmp[:].rearrange("p a b -> p (a b)"), smk[:, sgm:sgm + 1],
                                    ytl[:].rearrange("p a b -> p (a b)"), ALU.mult, ALU.add, [r_ytmp, r_smk, r_ytl], [r_ytl])
                        for s in range(4):
                            si = tt_ * 4 + s
                            xb = si % 2
                            x1 = xl[xb]
                            rx1 = r_xl[xb]
                            DMA("sync", x1[:], x_seg[t0 + s * 128:t0 + (s + 1) * 128, :], [], [rx1])
                            for dg in range(4):
                                pb, rp = genbank(POOL_C)
                                for kc in range(16):
                                    MM(pb[:], ytl[:, kc, s * 128:(s + 1) * 128], wo[dg][:, kc, :], kc == 0, kc == 15, [r_ytl, r_wo], [rp])
                                dsl = slice(dg * 512, (dg + 1) * 512)
                                tt(tmpc[:, dsl], pb[:], bc_g1[:, dsl], ALU.mult, [rp, r_bc], [r_tmpc])
                                tt(x1[:, dsl], x1[:, dsl], tmpc[:, dsl], ALU.add, [r_tmpc, rx1], [rx1])
                            DMA("sync", x1_d[t0 + s * 128:t0 + (s + 1) * 128, :], x1[:], [rx1], [r_x1d])
                            A(lambda e, x1=x1: e.activation(out=junk2[:], in_=x1[:], func=AF.Square, accum_out=ss2[:, 0:1]),
                              [rx1], [r_junk2, r_ss2])
                            ts(ss2[:, 1:2], ss2[:, 0:1], 1.0 / D, EPS, ALU.mult, ALU.add, [r_ss2], [r_ss2])
                            act(ss2[:, 1:2], ss2[:, 1:2], AF.Sqrt, [r_ss2], [r_ss2])
                            V(lambda e: e.reciprocal(out=ss2[:, 1:2], in_=ss2[:, 1:2]), [r_ss2], [r_ss2])
                            stt(tmpc[:], x1[:], ss2[:, 1:2], bc_s2[:], ALU.mult, ALU.mult, [rx1, r_ss2, r_bc], [r_tmpc])
                            hb_, rhb_ = hnb[si % 2], r_hnb[si % 2]
                            tt(hb_[:], tmpc[:], bc_h2[:], ALU.add, [r_tmpc, r_bc], [rhb_])
                            if DEBUG and si == 0:
                                DMA("sync", dbg["d_x1"], x1[:], [rx1], [])
                                DMA("sync", dbg["d_hn2"], hb_[:], [rhb_], [])
                            hp_, rhp_ = hnp[si % 2], r_hnp[si % 2]
                            P.add("gpsimd", lambda e, hp_=hp_, hb_=hb_: e.tensor_copy(
                                out=hp_[:].rearrange("t (kc p) -> t kc p", kc=16),
                                in_=hb_[:].rearrange("t (p kc) -> t kc p", kc=16)), [rhb_], [rhp_])
                            DMA("sync", hn2_d[t0 + s * 128:t0 + (s + 1) * 128, :], hp_[:], [rhp_], [r_hn2d])
                            for half in range(2):
                                for q in range(8):
                                    dc = half * 8 + q
                                    TR(ps_trs[half][:, q * 128:(q + 1) * 128], hb_[:, dc * 128:(dc + 1) * 128], ident_b[:],
                                       [rhb_, r_k], [r_ptr[half]])
                                act(hT16[:, half * 8:(half + 1) * 8, :], ps_trs[half][:].rearrange("p (q t) -> p q t", q=8), AF.Copy,
                                    [r_ptr[half]], [r_hT16])
                            pb, rp = genbank(POOL_C)
                            for dc in range(16):
                                MM(pb[:, 0:36], hT16[:, dc, :], wrt_b[:, dc, :], dc == 0, dc == 15, [r_hT16, r_wrt], [rp])
                            tt(lg[:], pb[:, 0:36], brt_t[:], ALU.add, [rp, r_brt], [r_lg])
                            V(lambda e: e.reduce_max(out=rt[:, 0:1], in_=lg[:, 0:4], axis=mybir.AxisListType.X), [r_lg], [r_rt])
                            ts(rt[:, 8:12], lg[:, 0:4], rt[:, 0:1], None, ALU.subtract, None, [r_lg, r_rt], [r_rt])
                            act(rt[:, 8:12], rt[:, 8:12], AF.Exp, [r_rt], [r_rt])
                            V(lambda e: e.reduce_sum(out=rt[:, 1:2], in_=rt[:, 8:12], axis=mybir.AxisListType.X), [r_rt], [r_rt])
                            V(lambda e: e.reciprocal(out=rt[:, 1:2], in_=rt[:, 1:2]), [r_rt], [r_rt])
                            ts(rt[:, 12:16], lg[:, 0:4], rt[:, 0:1], None, ALU.is_equal, None, [r_lg, r_rt], [r_rt])
                            ts(rt[:, 12:16], rt[:, 12:16], 1.0, 1e30, ALU.subtract, ALU.mult, [r_rt], [r_rt])
                            for g_ in range(4):
                                ts(rt[:, 16 + g_ * 8:24 + g_ * 8], lg[:, 4 + g_ * 8:12 + g_ * 8], rt[:, 12 + g_:13 + g_], None,
                                   ALU.add, None, [r_lg, r_rt], [r_rt])
                            EL = rt[:, 16:48]
                            V(lambda e: e.reduce_max(out=rt[:, 2:3], in_=rt[:, 16:48], axis=mybir.AxisListType.X), [r_rt], [r_rt])
                            ts(oh1s[:, si, :], EL, rt[:, 2:3], None, ALU.is_equal, None, [r_rt], [r_ohs])
                            stt(lg[:, 4:36], oh1s[:, si, :], -1e30, EL, ALU.mult, ALU.add, [r_ohs, r_rt], [r_lg])
                            V(lambda e: e.reduce_max(out=rt[:, 3:4], in_=lg[:, 4:36], axis=mybir.AxisListType.X), [r_lg], [r_rt])
                            ts(oh2s[:, si, :], lg[:, 4:36], rt[:, 3:4], None, ALU.is_equal, None, [r_lg, r_rt], [r_ohs])
                            tt(rt[:, 4:5], rt[:, 3:4], rt[:, 2:3], ALU.subtract, [r_rt], [r_rt])
                            act(rt[:, 4:5], rt[:, 4:5], AF.Exp, [r_rt], [r_rt])
                            ts(rt[:, 5:6], rt[:, 4:5], 1.0, None, ALU.add, None, [r_rt], [r_rt])
                            V(lambda e: e.reciprocal(out=rt[:, 5:6], in_=rt[:, 5:6]), [r_rt], [r_rt])
                            tt(rt[:, 6:7], rt[:, 4:5], rt[:, 5:6], ALU.mult, [r_rt], [r_rt])
                            tt(w12[:, si, 0:1], rt[:, 5:6], rt[:, 1:2], ALU.mult, [r_rt], [r_w12])
                            tt(w12[:, si, 1:2], rt[:, 6:7], rt[:, 1:2], ALU.mult, [r_rt], [r_w12])
                    P.barrier()
                with ExitStack() as esC2:
                    oh = SB(esC2, "oh", [128, NSUB, 32]); r_oh = R()
                    ohc = SB(esC2, "ohc", [128, 32]); r_ohc = R()
                    cnt = SB(esC2, "cnt", [128, 32]); r_cnt = R()
                    nbk = SB(esC2, "nbk", [128, 32]); r_nbk = R()
                    pend = SB(esC2, "pend", [128, 32]); r_pend = R()
                    base = SB(esC2, "base", [128, 32]); r_base = R()
                    tm3 = SB(esC2, "tm3", [128, NSUB, 32]); r_tm3 = R()
                    dst = SB(esC2, "dst", [128, 2 * NSUB]); r_dst = R()
                    ebk = SB(esC2, "ebk", [128, NBLK]); r_ebk = R()
                    pay = SB(esC2, "pay", [128, 2 * NSUB, 2]); r_pay = R()
                    zer = SB(esC2, "zer", [128, 128]); r_zer = R()
                    ohb = SB(esC2, "ohb", [128, NSUB, 32], BF16)
                    ohcb = SB(esC2, "ohcb", [128, 32], BF16); r_ohcb = R()
                    striub = SB(esC2, "striub", [128, 128], BF16)
                    V(lambda e: e.tensor_copy(out=striub[:], in_=striu[:]), [r_striu], [r_striu])
                    tt(oh[:], oh1s[:], oh2s[:], ALU.add, [r_ohs], [r_oh])
                    V(lambda e: e.tensor_copy(out=ohb[:], in_=oh[:]), [r_oh], [r_oh])
                    V(lambda e: e.memset(ohc[:], 0.0), [], [r_ohc])
                    V(lambda e: e.memset(ohcb[:], 0.0), [], [r_ohcb])
                    V(lambda e: e.memset(zer[:], 0.0), [], [r_zer])
                    DMA("sync", sinfo_d.rearrange("(p a) c -> p (a c)", p=128), zer[:], [r_zer], [r_sinfo])
                    for si in range(NSUB):
                        pb, rp = genbank(POOL_C)
                        MM(pb[:, 0:32], striub[:], ohb[:, si, :], True, False, [r_striu, r_oh], [rp])
                        MM(pb[:, 0:32], ones_b[:], ohcb[:], False, True, [r_k, r_ohcb], [rp])
                        act(rank[:, si, :], pb[:, 0:32], AF.Copy, [rp], [r_rank])
                        tt(ohc[:], ohc[:], oh[:, si, :], ALU.add, [r_ohc, r_oh], [r_ohc])
                        V(lambda e: e.tensor_copy(out=ohcb[:], in_=ohc[:]), [r_ohc], [r_ohcb])
                    pb, rp = genbank(POOL_C)
                    MM(pb[:, 0:32], ones_b[:], ohcb[:], True, True, [r_k, r_ohcb], [rp])
                    act(cnt[:], pb[:, 0:32], AF.Copy, [rp], [r_cnt])
                    V(lambda e: e.memset(nbk[:], 0.0), [], [r_nbk])
                    for k_ in range(16):
                        stt(nbk[:], cnt[:], float(128 * k_), nbk[:], ALU.is_gt, ALU.add, [r_cnt, r_nbk], [r_nbk])
                    V(lambda e: e.tensor_tensor_scan(out=pend[:], data0=ones_f[:, 0:32], data1=nbk[:], initial=0.0,
                                                     op0=ALU.mult, op1=ALU.add), [r_nbk, r_k], [r_pend])
                    tt(base[:], pend[:], nbk[:], ALU.subtract, [r_pend, r_nbk], [r_base])
                    ts(base[:], base[:], 128.0, None, ALU.mult, None, [r_base], [r_base])
                    for si in range(NSUB):
                        tt(tm3[:, si, :], rank[:, si, :], base[:], ALU.add, [r_rank, r_base], [r_tm3])
                    for k_, ohk in enumerate([oh1s, oh2s]):
                        tt(oh[:], tm3[:], ohk[:], ALU.mult, [r_tm3, r_ohs, r_oh], [r_oh])
                        for si in range(NSUB):
                            V(lambda e, si=si, k_=k_: e.reduce_sum(out=dst[:, 2 * si + k_:2 * si + k_ + 1], in_=oh[:, si, :],
                                                                  axis=mybir.AxisListType.X), [r_oh], [r_dst])
                    V(lambda e: e.tensor_copy(out=desti[:], in_=dst[:]), [r_dst], [r_desti])
                    V(lambda e: e.memset(ebk[:], 0.0), [], [r_ebk])
                    for e_ in range(32):
                        stt(ebk[:], cst2[:, 64:64 + NBLK], pend[:, e_:e_ + 1], ebk[:], ALU.is_ge, ALU.add, [r_cst, r_pend, r_ebk], [r_ebk])
                    ts(ebk[:], ebk[:], 31.0, 128.0, ALU.min, ALU.mult, [r_ebk], [r_ebk])
                    ts(ebk[:], ebk[:], cst2[:, 0:1], None, ALU.add, None, [r_ebk, r_cst], [r_ebk])
                    V(lambda e: e.tensor_copy(out=widx[:], in_=ebk[:]), [r_ebk], [r_widx])
                    for si in range(NSUB):
                        for k_ in range(2):
                            ts(pay[:, 2 * si + k_, 0:1], cst2[:, 0:1], float(si * 128), None, ALU.add, None, [r_cst], [r_pay])
                            V(lambda e, si=si, k_=k_: e.tensor_copy(out=pay[:, 2 * si + k_, 1:2], in_=w12[:, si, k_:k_ + 1]), [r_w12], [r_pay])
                    if DEBUG:
                        DMA("sync", dbg["d_dst"], dst[:], [r_dst], [])
                        DMA("sync", dbg["d_ebk"], ebk[:], [r_ebk], [])
                    for c_ in range(2 * NSUB):
                        sc_ = P.add("gpsimd", lambda e, c_=c_: e.indirect_dma_start(
                            out=sinfo_d[:, :], out_offset=bass.IndirectOffsetOnAxis(ap=desti[:, c_:c_ + 1], axis=0),
                            in_=pay[:, c_, :], in_offset=None), [r_pay, r_desti, r_sinfo], [r_sinfo2], dma=True)
                    P.barrier()
                with ExitStack() as esC4:
                    NW = 1
                    wst = [SB(esC4, f"wst{i}", [128, 8192]) for i in range(2)]; r_wst = [R(), R()]
                    wgs = [SB(esC4, f"wgs{i}", [128, 8192], BF16) for i in range(NW)]; r_wgs = [R() for _ in range(NW)]
                    wus = [SB(esC4, f"wus{i}", [128, 8192], BF16) for i in range(NW)]; r_wus = [R() for _ in range(NW)]
                    wds = [SB(esC4, f"wds{i}", [128, 8192], BF16) for i in range(NW)]; r_wds = [R() for _ in range(NW)]
                    sif = [SB(esC4, f"sif{i}", [128, 2]) for i in range(2)]; r_sif = [R(), R()]
                    sii = [SB(esC4, f"sii{i}", [128, 1], I32) for i in range(2)]; r_sii = [R(), R()]
                    xbr = [SB(esC4, f"xbr{i}", [128, D], BF16) for i in range(2)]; r_xbr = [R(), R()]
                    xbT = [SB(esC4, f"xbT{i}", [128, 16, 128], BF16) for i in range(2)]; r_xbT = [R(), R()]
                    sgt = SB(esC4, "sgt", [128, 512]); r_sgt = R()
                    hh = SB(esC4, "hh", [128, 512], BF16); r_hh = R()
                    hhT = SB(esC4, "hhT", [128, 4, 128], BF16); r_hhT = R()
                    ybr = [SB(esC4, f"ybr{i}", [128, D]) for i in range(2)]; r_ybr = [R(), R()]
                    def fetch_x(bb):
                        j2 = bb % 2
                        DMA("sync", sif[j2][:], sinfo_d[bb * 128:(bb + 1) * 128, :], [r_sinfo2], [r_sif[j2]])
                        V(lambda e, j2=j2: e.tensor_copy(out=sii[j2][:], in_=sif[j2][:, 0:1]), [r_sif[j2]], [r_sii[j2]])
                        P.add("gpsimd", lambda e, j2=j2: e.indirect_dma_start(
                            out=xbr[j2][:], out_offset=None, in_=hn2_d[:, :],
                            in_offset=bass.IndirectOffsetOnAxis(ap=sii[j2][:, 0:1], axis=0)), [r_sii[j2], r_hn2d], [r_xbr[j2]], dma=True)

                    fetch_x(0)
                    for b_ in range(NBLK_RUN):
                        i2 = b_ % 2
                        iw = b_ % NW
                        for m_, (wt, rw_, srcw, ceng) in enumerate(((wgs[iw], r_wgs[iw], weg2, "vector"), (wus[iw], r_wus[iw], weu2, "scalar"),
                                                                 (wds[iw], r_wds[iw], wed2, "vector"))):
                            kst = (3 * b_ + m_) % 2
                            P.add("gpsimd", lambda e, kst=kst, srcw=srcw, b_=b_: e.indirect_dma_start(
                                out=wst[kst][:], out_offset=None, in_=srcw[:, :],
                                in_offset=bass.IndirectOffsetOnAxis(ap=widx[:, b_:b_ + 1], axis=0)), [r_widx], [r_wst[kst]], dma=True)
                            if ceng == "vector":
                                V(lambda e, wt=wt, kst=kst: e.tensor_copy(out=wt[:], in_=wst[kst][:]), [r_wst[kst]], [rw_])
                            else:
                                act(wt[:], wst[kst][:], AF.Copy, [r_wst[kst]], [rw_])
                        if b_ + 1 < NBLK_RUN:
                            fetch_x(b_ + 1)
                        for half in range(2):
                            for q in range(8):
                                kc = half * 8 + q
                                TR(ps_trs[half][:, q * 128:(q + 1) * 128], xbr[i2][:, kc * 128:(kc + 1) * 128], ident_b[:],
                                   [r_xbr[i2], r_k], [r_ptr[half]])
                            act(xbT[i2][:, half * 8:(half + 1) * 8, :], ps_trs[half][:].rearrange("p (q t) -> p q t", q=8), AF.Copy,
                                [r_ptr[half]], [r_xbT[i2]])
                        wgv = wgs[iw][:].rearrange("p (kc n) -> p kc n", kc=16)
                        wuv = wus[iw][:].rearrange("p (kc n) -> p kc n", kc=16)
                        wdv = wds[iw][:].rearrange("p (fc n) -> p fc n", fc=4)
                        pg_, rpg = genbank(POOL_C)
                        for kc in range(16):
                            MM(pg_[:], xbT[i2][:, kc, :], wgv[:, kc, :], kc == 0, kc == 15, [r_xbT[i2], r_wgs[iw]], [rpg])
                        pu_, rpu = genbank(POOL_C)
                        for kc in range(16):
                            MM(pu_[:], xbT[i2][:, kc, :], wuv[:, kc, :], kc == 0, kc == 15, [r_xbT[i2], r_wus[iw]], [rpu])
                        act(sgt[:], pg_[:], AF.Silu, [rpg], [r_sgt])
                        tt(hh[:], sgt[:], pu_[:], ALU.mult, [r_sgt, rpu], [r_hh])
                        for fc in range(4):
                            TR(ps_trs[0][:, fc * 128:(fc + 1) * 128], hh[:, fc:512:4], ident_b[:], [r_hh, r_k], [r_ptr[0]])
                        act(hhT[:], ps_trs[0][:, 0:512].rearrange("p (q t) -> p q t", q=4), AF.Copy, [r_ptr[0]], [r_hhT])
                        yb_, ryb_ = ybr[i2], r_ybr[i2]
                        for dg in range(4):
                            po, rpo = genbank(POOL_C)
                            for fc in range(4):
                                MM(po[:], hhT[:, fc, :], wdv[:, fc, dg * 512:(dg + 1) * 512], fc == 0, fc == 3, [r_hhT, r_wds[iw]], [rpo])
                            act(yb_[:, dg * 512:(dg + 1) * 512], po[:], AF.Identity, [rpo, r_sif[i2]], [ryb_], scale=sif[i2][:, 1:2])
                        DMA("sync", yb_d[b_ * 128:(b_ + 1) * 128, :], yb_[:], [ryb_], [r_ybd])
                    P.barrier()
                with ExitStack() as esC5:
                    y1 = [SB(esC5, f"y1_{i}", [128, D]) for i in range(2)]; r_y1 = [R(), R()]
                    y2 = [SB(esC5, f"y2_{i}", [128, D]) for i in range(2)]; r_y2 = [R(), R()]
                    x1r = [SB(esC5, f"x1r{i}", [128, D]) for i in range(2)]; r_x1r = [R(), R()]
                    junk3 = SB(esC5, "junk3", [128, D], BF16); r_junk3 = R()
                    ss3 = SB(esC5, "ss3", [128, 4]); r_ss3 = R()
                    for si in range(NSUB):
                        i2 = si % 2
                        P.add("gpsimd", lambda e, i2=i2, si=si: e.indirect_dma_start(
                            out=y1[i2][:], out_offset=None, in_=yb_d[:, :],
                            in_offset=bass.IndirectOffsetOnAxis(ap=desti[:, 2 * si:2 * si + 1], axis=0)), [r_desti, r_ybd], [r_y1[i2]], dma=True)
                        P.add("gpsimd", lambda e, i2=i2, si=si: e.indirect_dma_start(
                            out=y2[i2][:], out_offset=None, in_=yb_d[:, :],
                            in_offset=bass.IndirectOffsetOnAxis(ap=desti[:, 2 * si + 1:2 * si + 2], axis=0)), [r_desti, r_ybd], [r_y2[i2]], dma=True)
                        DMA("sync", x1r[i2][:], x1_d[si * 128:(si + 1) * 128, :], [r_x1d], [r_x1r[i2]])
                        tt(y1[i2][:], y1[i2][:], y2[i2][:], ALU.add, [r_y1[i2], r_y2[i2]], [r_y1[i2]])
                        tt(y1[i2][:], y1[i2][:], bc_g2[:], ALU.mult, [r_y1[i2], r_bc], [r_y1[i2]])
                        tt(x1r[i2][:], x1r[i2][:], y1[i2][:], ALU.add, [r_x1r[i2], r_y1[i2]], [r_x1r[i2]])
                        A(lambda e, i2=i2: e.activation(out=junk3[:], in_=x1r[i2][:], func=AF.Square, accum_out=ss3[:, 0:1]),
                          [r_x1r[i2]], [r_junk3, r_ss3])
                        ts(ss3[:, 1:2], ss3[:, 0:1], 1.0 / D, EPS, ALU.mult, ALU.add, [r_ss3], [r_ss3])
                        act(ss3[:, 1:2], ss3[:, 1:2], AF.Sqrt, [r_ss3], [r_ss3])
                        V(lambda e: e.reciprocal(out=ss3[:, 1:2], in_=ss3[:, 1:2]), [r_ss3], [r_ss3])
                        stt(y2[i2][:], x1r[i2][:], ss3[:, 1:2], bc_fg[:], ALU.mult, ALU.mult, [r_x1r[i2], r_ss3, r_bc, r_y2[i2]], [r_y2[i2]])
                        DMA("sync", out[si * 128:(si + 1) * 128, :], y2[i2][:], [r_y2[i2]], [])
        finally:
            HALT[0] = False
        fin = P.add("sync", None)
        fin.deps = {o.idx for o in P.ops if o.is_dma}

        with nc.Block() as block:
            run = P.finalize(sems, dsems)

            @block.sync
            def _(e):
                run("sync", e)

            @block.scalar
            def _(e):
                run("scalar", e)

            @block.vector
            def _(e):
                run("vector", e)

            @block.gpsimd
            def _(e):
                run("gpsimd", e)

            @block.tensor
            def _(e):
                run("tensor", e)
    return nc


def _col(v, n):
    return np.ascontiguousarray(np.asarray(v, np.float32).reshape(n, 128).T)


def make_in_maps(I):
    f = lambda a: np.asarray(a, np.float32)
    x, c = f(I["x"]), f(I["c"])
    w_in = f(I["w_in"])[0]
    bg = f(I["b_gates"])[0]
    conv_qk = f(I["conv_qk"])[0]
    lcw = f(I["lru_conv_w"])[0]
    consts = np.zeros((128, 256), np.float32)
    consts[:, 0:128] = np.eye(128, dtype=np.float32)
    consts[:, 128:256] = np.triu(np.ones((128, 128), np.float32))
    wout = f(I["w_out"])[0]
    perm = []
    for r in range(4):
        perm += list(range(r * 256, (r + 1) * 256)) + list(range(1024 + r * 256, 1024 + (r + 1) * 256))
    wout_p = np.ascontiguousarray(wout[perm, :])
    wrt = np.ascontiguousarray(np.concatenate([f(I["w_group"])[0], f(I["w_router"])[0]], axis=1))
    brt = np.ascontiguousarray(np.broadcast_to(np.concatenate([f(I["b_group"])[0], f(I["b_router"])[0]])[None, :], (128, 36)))
    w_ada = np.ascontiguousarray(f(I["w_ada"])[0])
    bada_c = _col(f(I["b_ada"])[0], 96)
    weg = np.ascontiguousarray(f(I["w_e_gate"])[0]).reshape(NE * 128, 8192)
    weu = np.ascontiguousarray(f(I["w_e_up"])[0]).reshape(NE * 128, 8192)
    wed = np.ascontiguousarray(f(I["w_e_down"])[0]).reshape(NE * 128, 8192)
    fing_bc = np.ascontiguousarray(np.broadcast_to(f(I["final_g"])[None, :], (128, D)))
    consts2 = np.zeros((128, 128), np.float32)
    consts2[:, 0] = np.arange(128)
    consts2[:, 64:128] = np.arange(64)[None, :]
    fing = _col(f(I["final_g"]), 16)
    maps = []
    for core in range(8):
        b, j = core // 4, core % 4
        q0, k0, v0, o0 = j * 256, 1024 + j * 256, 2048 + j * 256, 3072 + j * 256
        ii, ff = 4096 + j, 4100 + j
        xr0, gr0 = 4104 + j * 256, 4104 + 1024 + j * 256
        cols = (list(range(q0, q0 + 256)) + list(range(k0, k0 + 256)) + list(range(o0, o0 + 256)) +
                list(range(xr0, xr0 + 256)) + list(range(gr0, gr0 + 256)) + [ii] * 128 + [ff] * 128 +
                list(range(v0, v0 + 256)))
        win = np.ascontiguousarray(w_in[:, cols])
        convw = np.zeros((128, 24), np.float32)
        for cb in range(6):
            if cb < 2:
                src = conv_qk[:, j * 256 + cb * 128: j * 256 + (cb + 1) * 128]
            elif cb < 4:
                src = conv_qk[:, 1024 + j * 256 + (cb - 2) * 128: 1024 + j * 256 + (cb - 1) * 128]
            else:
                src = lcw[:, j * 256 + (cb - 4) * 128: j * 256 + (cb - 3) * 128]
            convw[:, cb * 4:(cb + 1) * 4] = src.T
        vecs = np.zeros((128, 16), np.float32)
        r0 = j * 256
        vecs[:, 0:2] = _col(f(I["lru_conv_b"])[0][r0:r0 + 256], 2)
        vecs[:, 2:4] = _col(f(I["b_lru_a"])[0][r0:r0 + 256], 2)
        vecs[:, 4:6] = _col(f(I["b_lru_x"])[0][r0:r0 + 256], 2)
        vecs[:, 6:8] = _col(f(I["lru_lambda"])[0][r0:r0 + 256], 2)
        vecs[:, 8:10] = _col(f(I["lru_norm_g"])[0][r0:r0 + 256], 2)
        vecs[:, 10:12] = _col(f(I["mh_norm_g"])[0][r0:r0 + 256], 2)
        vecs[:, 12] = bg[j]
        vecs[:, 13] = bg[4 + j]
        wl = np.zeros((128, 512), np.float32)
        for bi in range(2):
            wl[:, bi * 128:(bi + 1) * 128] = f(I["w_lru_a"])[0][2 * j + bi]
            wl[:, (2 + bi) * 128:(3 + bi) * 128] = f(I["w_lru_x"])[0][2 * j + bi]
        segmask = np.zeros((128, 4), np.float32)
        segmask[:, j] = 1.0
        maps.append({
            "x_full": np.ascontiguousarray(x[b]), "x_seg": np.ascontiguousarray(x[b, j * 2048:(j + 1) * 2048]),
            "c_col": _col(c[b], 16), "w_ada": w_ada, "bada_c": bada_c, "win": win, "convw": convw, "vecs": vecs,
            "wlru": wl, "wout": wout_p, "wrt": wrt, "brt": brt, "weg": weg, "weu": weu, "wed": wed, "fing": fing,
            "consts": consts, "segmask": segmask, "fing_bc": fing_bc, "consts2": consts2,
        })
    return maps


_NC = None
LAST = None


def kernel(**inputs):
    global _NC, LAST
    maps = make_in_maps(inputs)
    nc = build_nc()
    res = run_bass_kernel_spmd(nc, maps, core_ids=list(range(8)))
    LAST = res
    out = np.zeros((2, S, D), np.float32)
    for core in range(8):
        b, j = core // 4, core % 4
        out[b, j * 2048:(j + 1) * 2048] = np.asarray(res.results[core]["out"], np.float32)
    return out
```
